# Optimizing a Trainium2 kernel written in Bass

```python
import math
import jax, jax.numpy as jnp
from jax import lax
import numpy as np

D_MODEL = 2048
BATCH = 8
SEQ = 2048
DEPTH = 1
DEC_BATCH = 8
DEC_SEQ = 16
PAST_LEN = 2048

CHUNK = 64
D_MIX = D_MODEL
D_POOL = D_MIX // 2
POOL_WINDOWS = (2, 4, 8, 16)
N_POOL_GROUPS = len(POOL_WINDOWS)
POOL_CH = D_POOL // N_POOL_GROUPS
POOL_HIST = max(POOL_WINDOWS) - 1
D_SSM = D_MIX - D_POOL
SSM_CH = 16
N_SSM_GROUPS = D_SSM // SSM_CH
SSM_STATE = 64
N_EXPERTS = 32
TOP_K = 4
D_FF = D_MODEL
SWIGLU_LIMIT = 7.0
SWIGLU_ALPHA = 1.702
MOE_BLOCK = 256
DN_ALPHA = (2 * DEPTH) ** 0.25
DN_BETA = (8 * DEPTH) ** -0.25
LN_EPS = 1e-5
F32 = jnp.float32

kernel_name = 'hybrid_pool_s5_moe_stream_step'


def _layer_norm(x, g, b):
    xf = x.astype(F32)
    mu = jnp.mean(xf, axis=-1, keepdims=True)
    var = jnp.mean(jnp.square(xf - mu), axis=-1, keepdims=True)
    return ((xf - mu) * lax.rsqrt(var + LN_EPS) * g.astype(F32) + b.astype(F32)).astype(x.dtype)


def _pool_mixer(u, u_hist, pos0, w_pool, pool_scale):
    bsz, seq, _ = u.shape
    ext = jnp.concatenate([u_hist.astype(F32), u.astype(F32)], axis=1)
    cs = jnp.concatenate([jnp.zeros((bsz, 1, D_POOL), F32), jnp.cumsum(ext, axis=1)], axis=1)
    hi = cs[:, POOL_HIST + 1:POOL_HIST + 1 + seq]
    pos = (pos0 + jnp.arange(seq)).astype(F32)[None, :, None]
    pooled = []
    for g, w in enumerate(POOL_WINDOWS):
        sl = slice(g * POOL_CH, (g + 1) * POOL_CH)
        lo = cs[:, POOL_HIST + 1 - w:POOL_HIST + 1 - w + seq, sl]
        cnt = jnp.minimum(pos + 1.0, float(w))
        pooled.append((hi[..., sl] - lo) / cnt)
    diff = jnp.concatenate(pooled, axis=-1) - ext[:, POOL_HIST:]
    diff = diff.reshape(bsz, seq, N_POOL_GROUPS, POOL_CH)
    y = jnp.einsum('blgc,gcd->blgd', diff, w_pool.astype(F32)).reshape(bsz, seq, D_POOL)
    y = y * pool_scale.astype(F32)
    return y.astype(u.dtype), ext[:, -POOL_HIST:].astype(u.dtype)


def _ssm_mixer(u, h0_re, h0_im, lambda_re, lambda_im, log_dt, b_re, b_im, c_re, c_im, d_skip, w_glu, b_glu):
    bsz, seq, _ = u.shape
    uf = u.astype(F32).reshape(bsz, seq, N_SSM_GROUPS, SSM_CH)
    lr, li = lambda_re.astype(F32), lambda_im.astype(F32)
    dt = jnp.exp(log_dt.astype(F32))[:, None]
    mag = jnp.exp(lr * dt)
    abar_re, abar_im = mag * jnp.cos(li * dt), mag * jnp.sin(li * dt)
    nr, ni = abar_re - 1.0, abar_im
    den = lr * lr + li * li
    k_re = (nr * lr + ni * li) / den
    k_im = (ni * lr - nr * li) / den
    br, bi = b_re.astype(F32), b_im.astype(F32)
    bb_re = k_re[..., None] * br - k_im[..., None] * bi
    bb_im = k_re[..., None] * bi + k_im[..., None] * br
    bu_re = jnp.einsum('blgc,gpc->blgp', uf, bb_re)
    bu_im = jnp.einsum('blgc,gpc->blgp', uf, bb_im)
    a_re = jnp.broadcast_to(abar_re[None, None], (1, seq, N_SSM_GROUPS, SSM_STATE))
    a_im = jnp.broadcast_to(abar_im[None, None], (1, seq, N_SSM_GROUPS, SSM_STATE))

    def combine(e1, e2):
        a1r, a1i, b1r, b1i = e1
        a2r, a2i, b2r, b2i = e2
        return (a2r * a1r - a2i * a1i, a2r * a1i + a2i * a1r,
                a2r * b1r - a2i * b1i + b2r, a2r * b1i + a2i * b1r + b2i)

    acc_re, acc_im, h_re, h_im = lax.associative_scan(combine, (a_re, a_im, bu_re, bu_im), axis=1)
    if h0_re is not None:
        s_re, s_im = h0_re.astype(F32)[:, None], h0_im.astype(F32)[:, None]
        h_re = h_re + acc_re * s_re - acc_im * s_im
        h_im = h_im + acc_re * s_im + acc_im * s_re
    y = (jnp.einsum('blgp,gcp->blgc', h_re, c_re.astype(F32))
         - jnp.einsum('blgp,gcp->blgc', h_im, c_im.astype(F32))
         + d_skip.astype(F32) * uf)
    y = jax.nn.gelu(y)
    y = y * jax.nn.sigmoid(jnp.einsum('blgc,gce->blge', y, w_glu.astype(F32)) + b_glu.astype(F32))
    return (y.reshape(bsz, seq, D_SSM).astype(u.dtype),
            h_re[:, -1].astype(u.dtype), h_im[:, -1].astype(u.dtype))


def _moe_block_rows(n_assign):
    target = -(-n_assign // N_EXPERTS)
    rows = 8
    while rows < min(target, MOE_BLOCK):
        rows *= 2
    return rows


def _moe(h, w_router, b_router, w_gate_up, b_gate_up, w_down, b_down):
    bsz, seq, d = h.shape
    n_tok = bsz * seq
    hf = h.reshape(n_tok, d)
    logits = (hf @ w_router).astype(F32) + b_router.astype(F32)
    top_val, top_idx = lax.top_k(logits, TOP_K)
    gates = jax.nn.softmax(top_val, axis=-1)
    n_assign = n_tok * TOP_K
    blk = _moe_block_rows(n_assign)
    n_blk = -(-n_assign // blk) + N_EXPERTS
    flat_e = top_idx.reshape(n_assign)
    flat_tok = jnp.arange(n_assign, dtype=jnp.int32) // TOP_K
    order = jnp.argsort(flat_e)
    sorted_e = flat_e[order]
    counts = jnp.bincount(flat_e, length=N_EXPERTS)
    padded = (counts + blk - 1) // blk * blk
    pad_end = jnp.cumsum(padded)
    pad_start = pad_end - padded
    start = jnp.cumsum(counts) - counts
    dest = pad_start[sorted_e] + (jnp.arange(n_assign) - start[sorted_e])
    n_rows = n_blk * blk
    row_tok = jnp.full((n_rows,), n_tok, jnp.int32).at[dest].set(flat_tok[order])
    row_gate = jnp.zeros((n_rows,), F32).at[dest].set(gates.reshape(n_assign)[order])
    blk_start = jnp.arange(n_blk) * blk
    blk_expert = jnp.minimum(jnp.sum(blk_start[:, None] >= pad_end[None, :], axis=1), N_EXPERTS - 1)
    xs = jnp.concatenate([hf, jnp.zeros((1, d), hf.dtype)], axis=0)[row_tok].reshape(n_blk, blk, d)

    def expert_block(args):
        xb, e = args
        gu = xb @ w_gate_up[e] + b_gate_up[e]
        gate = jnp.minimum(gu[:, :D_FF], SWIGLU_LIMIT)
        up = jnp.clip(gu[:, D_FF:], -SWIGLU_LIMIT, SWIGLU_LIMIT)
        act = (up + 1.0) * (gate * jax.nn.sigmoid(SWIGLU_ALPHA * gate))
        return act @ w_down[e] + b_down[e]

    ys = lax.map(expert_block, (xs, blk_expert)).reshape(n_rows, d)
    out = jnp.zeros((n_tok + 1, d), F32).at[row_tok].add(ys.astype(F32) * row_gate[:, None])[:n_tok]
    return out.reshape(bsz, seq, d).astype(h.dtype)


def _encoder_layer(x, c, pool_hist, h0_re, h0_im, pos0, lw):
    mod = (jax.nn.silu(c) @ lw['w_ada'] + lw['b_ada'])[:, None, :]
    sh1, sc1, g1, sh2, sc2, g2 = jnp.split(mod, 6, axis=-1)
    h = x * (1.0 + sc1) + sh1
    u = h @ lw['w_in']
    y_pool, new_hist = _pool_mixer(u[..., :D_POOL], pool_hist, pos0, lw['w_pool'], lw['pool_scale'])
    y_ssm, s_re, s_im = _ssm_mixer(u[..., D_POOL:], h0_re, h0_im, lw['lambda_re'], lw['lambda_im'],
                                   lw['log_dt'], lw['ssm_b_re'], lw['ssm_b_im'], lw['ssm_c_re'],
                                   lw['ssm_c_im'], lw['d_skip'], lw['w_glu'], lw['b_glu'])
    mix = jnp.concatenate([y_pool, y_ssm], axis=-1) @ lw['w_out']
    x = _layer_norm(DN_ALPHA * x + g1 * mix, lw['ln1_g'], lw['ln1_b'])
    h = x * (1.0 + sc2) + sh2
    ffn = _moe(h, lw['w_router'], lw['b_router'], lw['w_gate_up'], lw['b_gate_up'], lw['w_down'], lw['b_down'])
    x = _layer_norm(DN_ALPHA * x + g2 * ffn, lw['ln2_g'], lw['ln2_b'])
    return x, new_hist, s_re, s_im


def setup_inputs(seed: int = 0) -> dict:
    key = jax.random.key(seed)
    ks = iter(jax.random.split(key, 40))

    def nrm(shape, scale):
        return jax.random.normal(next(ks), shape, F32) * scale

    G, P, C = N_SSM_GROUPS, SSM_STATE, SSM_CH
    return {
        'x_prompt': nrm((BATCH, SEQ, D_MODEL), 1.0),
        'x_sample': nrm((DEC_BATCH, DEC_SEQ, D_MODEL), 1.0),
        'cache_pool': nrm((DEPTH, DEC_BATCH, POOL_HIST, D_POOL), 1.0),
        'state_ssm_re': nrm((DEPTH, DEC_BATCH, G, P), 0.1),
        'state_ssm_im': nrm((DEPTH, DEC_BATCH, G, P), 0.1),
        'c_prompt': nrm((BATCH, D_MODEL), 1.0),
        'c_sample': nrm((DEC_BATCH, D_MODEL), 1.0),
        'w_ada': nrm((DEPTH, D_MODEL, 6 * D_MODEL), 0.5 * D_MODEL ** -0.5),
        'b_ada': nrm((DEPTH, 6 * D_MODEL), 0.01),
        'w_in': nrm((DEPTH, D_MODEL, D_MIX), D_MODEL ** -0.5),
        'w_pool': nrm((DEPTH, N_POOL_GROUPS, POOL_CH, POOL_CH), POOL_CH ** -0.5),
        'pool_scale': 1.0 + nrm((DEPTH, D_POOL), 0.1),
        'lambda_re': -0.5 + nrm((DEPTH, G, P), 0.01),
        'lambda_im': math.pi * jnp.arange(P, dtype=F32)[None, None, :] + nrm((DEPTH, G, P), 0.01),
        'log_dt': jax.random.uniform(next(ks), (DEPTH, G), F32, math.log(1e-3), math.log(1e-1)),
        'ssm_b_re': nrm((DEPTH, G, P, C), (2 * C) ** -0.5),
        'ssm_b_im': nrm((DEPTH, G, P, C), (2 * C) ** -0.5),
        'ssm_c_re': nrm((DEPTH, G, C, P), (2 * P) ** -0.5),
        'ssm_c_im': nrm((DEPTH, G, C, P), (2 * P) ** -0.5),
        'd_skip': nrm((DEPTH, G, C), 1.0),
        'w_glu': nrm((DEPTH, G, C, C), C ** -0.5),
        'b_glu': nrm((DEPTH, G, C), 0.01),
        'w_out': nrm((DEPTH, D_MIX, D_MODEL), DN_BETA * D_MIX ** -0.5),
        'ln1_g': 1.0 + nrm((DEPTH, D_MODEL), 0.01),
        'ln1_b': nrm((DEPTH, D_MODEL), 0.01),
        'w_router': nrm((DEPTH, D_MODEL, N_EXPERTS), D_MODEL ** -0.5),
        'b_router': nrm((DEPTH, N_EXPERTS), 0.01),
        'w_gate_up': nrm((DEPTH, N_EXPERTS, D_MODEL, 2 * D_FF), D_MODEL ** -0.5),
        'b_gate_up': nrm((DEPTH, N_EXPERTS, 2 * D_FF), 0.01),
        'w_down': nrm((DEPTH, N_EXPERTS, D_FF, D_MODEL), DN_BETA * D_FF ** -0.5),
        'b_down': nrm((DEPTH, N_EXPERTS, D_MODEL), 0.01),
        'ln2_g': 1.0 + nrm((DEPTH, D_MODEL), 0.01),
        'ln2_b': nrm((DEPTH, D_MODEL), 0.01),
    }


def reference(x_prompt, x_sample, cache_pool, state_ssm_re, state_ssm_im, c_prompt, c_sample,
              w_ada, b_ada, w_in, w_pool, pool_scale, lambda_re, lambda_im, log_dt,
              ssm_b_re, ssm_b_im, ssm_c_re, ssm_c_im, d_skip, w_glu, b_glu, w_out, ln1_g, ln1_b,
              w_router, b_router, w_gate_up, b_gate_up, w_down, b_down, ln2_g, ln2_b):
    xp, xs = x_prompt, x_sample
    pool_p, re_p, im_p, pool_s, re_s, im_s = [], [], [], [], [], []
    for l in range(DEPTH):
        lw = {
            'w_ada': w_ada[l], 'b_ada': b_ada[l], 'w_in': w_in[l], 'w_pool': w_pool[l],
            'pool_scale': pool_scale[l], 'lambda_re': lambda_re[l], 'lambda_im': lambda_im[l],
            'log_dt': log_dt[l], 'ssm_b_re': ssm_b_re[l], 'ssm_b_im': ssm_b_im[l],
            'ssm_c_re': ssm_c_re[l], 'ssm_c_im': ssm_c_im[l], 'd_skip': d_skip[l],
            'w_glu': w_glu[l], 'b_glu': b_glu[l], 'w_out': w_out[l], 'ln1_g': ln1_g[l], 'ln1_b': ln1_b[l],
            'w_router': w_router[l], 'b_router': b_router[l], 'w_gate_up': w_gate_up[l],
            'b_gate_up': b_gate_up[l], 'w_down': w_down[l], 'b_down': b_down[l],
            'ln2_g': ln2_g[l], 'ln2_b': ln2_b[l],
        }
        empty_hist = jnp.zeros((xp.shape[0], POOL_HIST, D_POOL), xp.dtype)
        xp, hp, rp, ip = _encoder_layer(xp, c_prompt, empty_hist, None, None, 0, lw)
        xs, hs, rs, is_ = _encoder_layer(xs, c_sample, cache_pool[l], state_ssm_re[l], state_ssm_im[l],
                                         PAST_LEN, lw)
        pool_p.append(hp); re_p.append(rp); im_p.append(ip)
        pool_s.append(hs); re_s.append(rs); im_s.append(is_)
    return (xp, xs, jnp.stack(pool_p), jnp.stack(re_p), jnp.stack(im_p),
            jnp.stack(pool_s), jnp.stack(re_s), jnp.stack(im_s))
```

```python
import numpy as np
from contextlib import ExitStack
import concourse.bass as bass
import concourse.mybir as mybir
from concourse.bass_utils import run_bass_kernel_spmd

F32 = mybir.dt.float32
BF16 = mybir.dt.bfloat16
ALU = mybir.AluOpType
AF = mybir.ActivationFunctionType

D = 2048
NP_ = 2048
NS_ = 16
TOK = NP_ + NS_
NE = 32
DN_ALPHA = 2.0 ** 0.25
LN_EPS = 1e-5
MAGIC = 12582912.0
CHUNKS = [(0, 512), (512, 512), (1024, 512), (1536, 512), (2048, 16)]
TT = [(i * 128, 128) for i in range(16)] + [(2048, 16)]
POOL_W = (2, 4, 8, 16)
DEBUG = False
KNOB = {}


class Prog:
    def __init__(self, nc, self_sync=True):
        self.nc = nc
        self.ops = []
        self.last_w = {}
        self.readers = {}
        self.self_sync = self_sync
        self.last_eng = {}
        self.last_dma = {}
        self.bar_pending = {}

    def barrier(self):
        snap = set(self.last_eng.values()) | set(self.last_dma.values())
        for e in ('pe', 'act', 'dve', 'pool', 'sp'):
            self.bar_pending[e] = set(snap) | self.bar_pending.get(e, set())

    def add(self, eng, fn, reads=(), writes=(), dma=0, semkey=None, indep=False):
        i = len(self.ops)
        deps = set()
        if eng in self.bar_pending:
            deps |= self.bar_pending.pop(eng)
        if dma:
            self.last_dma[semkey] = i
        else:
            self.last_eng[eng] = i
        for k in reads:
            j = self.last_w.get(k)
            if j is not None:
                deps.add(j)
        for k in writes:
            j = self.last_w.get(k)
            if j is not None:
                deps.add(j)
            rd = self.readers.get(k)
            if rd:
                deps.update(rd.values())
        rkey = ('dma', semkey) if dma else ('eng', eng)
        for k in reads:
            self.readers.setdefault(k, {})[rkey] = i
        for k in writes:
            self.last_w[k] = i
            self.readers[k] = {}
        self.ops.append(dict(eng=eng, fn=fn, deps=deps, dma=dma, semkey=semkey, indep=indep))
        return i

    def pe(self, fn, reads=(), writes=()):
        return self.add('pe', fn, reads, writes)

    def act(self, fn, reads=(), writes=(), indep=False):
        return self.add('act', fn, reads, writes, indep=indep)

    def dve(self, fn, reads=(), writes=(), indep=False):
        return self.add('dve', fn, reads, writes, indep=indep)

    def pool(self, fn, reads=(), writes=(), indep=False):
        return self.add('pool', fn, reads, writes, indep=indep)

    def dma(self, q, fn, n, semkey, reads=(), writes=()):
        return self.add(q, fn, reads, writes, dma=n, semkey=semkey)

    def finalize(self, final_reads=()):
        nc = self.nc
        ops = self.ops
        self.add('sp', None, reads=final_reads)
        needed = [False] * len(ops)
        for i, o in enumerate(ops):
            for j in o['deps']:
                p = ops[j]
                if p['dma']:
                    continue
                if p['eng'] == o['eng'] and (o['eng'] == 'pe' or not self.self_sync):
                    continue
                needed[j] = True
        ms = {}
        ms_base = {}
        ms_cnt = {e: 0 for e in ('pe', 'act', 'dve', 'pool', 'sp')}
        dma_cnt = {}
        tok = {}

        class _Dummy:
            def __getattr__(self, name):
                return lambda *a, **k: _Dummy()

        for i, o in enumerate(ops):
            if o['dma']:
                c = dma_cnt.get(o['semkey'], 0) + 16 * o['dma']
                dma_cnt[o['semkey']] = c
                tok[i] = c
                continue
            nsub = 1
            if o['fn'] is not None and o['eng'] != 'pe' and self.self_sync and not o.get('indep'):
                r = o['fn'](_Dummy())
                nsub = len(r) if isinstance(r, (list, tuple)) else 1
            if nsub > 1:
                ms_base[i] = ms_cnt[o['eng']]
                ms_cnt[o['eng']] += nsub
                ms[i] = ms_cnt[o['eng']]
                o['nsub'] = nsub
            elif needed[i]:
                ms_cnt[o['eng']] += 1
                ms[i] = ms_cnt[o['eng']]
        with ExitStack() as st:
            esem = {e: st.enter_context(nc.semaphore('s_' + e)) for e in ms_cnt}
            dsem = {}
            for k in dma_cnt:
                dsem[k] = st.enter_context(nc.semaphore('d%d' % len(dsem)))
            block = st.enter_context(nc.Block())

            def emit(ename, eng):
                waited = {}
                for i, o in enumerate(ops):
                    if o['eng'] != ename:
                        continue
                    need = {}
                    for j in o['deps']:
                        p = ops[j]
                        if p['dma']:
                            s = dsem[p['semkey']]
                            v = tok[j]
                        else:
                            if p['eng'] == ename and (ename == 'pe' or not self.self_sync):
                                continue
                            s = esem[p['eng']]
                            v = ms[j]
                        if need.get(s, 0) < v:
                            need[s] = v
                    for s, v in need.items():
                        if waited.get(s, 0) < v:
                            eng.wait_ge(s, v)
                            waited[s] = v
                    if o['fn'] is None:
                        continue
                    if o.get('nsub'):
                        sem_e = esem[ename]
                        base = ms_base[i]
                        st_ = {'k': 0}

                        class _Proxy:
                            def __getattr__(self_, name):
                                real = getattr(eng, name)

                                def wrapped(*a, **kw):
                                    if st_['k'] > 0:
                                        eng.wait_ge(sem_e, base + st_['k'])
                                    ins = real(*a, **kw)
                                    ins.then_inc(sem_e, 1)
                                    st_['k'] += 1
                                    return ins
                                return wrapped
                        o['fn'](_Proxy())
                        assert st_['k'] == o['nsub'], (st_['k'], o['nsub'])
                        waited[sem_e] = max(waited.get(sem_e, 0), base + o['nsub'] - 1)
                        continue
                    r = o['fn'](eng)
                    if o['dma']:
                        assert isinstance(r, (list, tuple)) and len(r) == o['dma'], (len(r), o['dma'])
                        for ins in r:
                            ins.then_inc(dsem[o['semkey']], 16)
                    elif needed[i]:
                        if isinstance(r, (list, tuple)):
                            r = r[-1]
                        r.then_inc(esem[ename], 1)

            @block.tensor
            def _(e):
                emit('pe', e)

            @block.scalar
            def _(e):
                emit('act', e)

            @block.vector
            def _(e):
                emit('dve', e)

            @block.gpsimd
            def _(e):
                emit('pool', e)

            @block.sync
            def _(e):
                emit('sp', e)


def build_nc(debug=False, with_moe=True, stop_after=None):
    nc = bass.Bass("TRN2", target_bir_lowering=False)

    def din(name, shape, dt=F32):
        return nc.dram_tensor(name, list(shape), dt, kind="ExternalInput").ap()

    def dout(name, shape, dt=F32):
        return nc.dram_tensor(name, list(shape), dt, kind="ExternalOutput").ap()

    def dscr(name, shape, dt=F32):
        return nc.dram_tensor(name, list(shape), dt, kind="Internal").ap()

    xT = din("xT", [D, TOK])
    xtok = din("xtok", [TOK, D])
    cT = din("cT", [128, 16, 2])
    w_ada = din("w_ada", [D, 6 * D])
    b_adaT = din("b_adaT", [128, 96])
    b_ada_row = din("b_ada_row", [1, 6 * D])
    w_in = din("w_in", [D, D])
    w_out = din("w_out", [D, D])
    w_pool = din("w_pool", [4, 256, 256])
    pool_scaleT = din("pool_scaleT", [128, 8])
    cache_poolT = din("cache_poolT", [1024, 15])
    lamst = din("lamst", [128, 2, 64])
    logdt_row = din("logdt_row", [1, 64])
    Bst = din("Bst", [2, 128, 1024])
    Cst = din("Cst", [2, 128, 1024])
    h0st = din("h0st", [128, 64])
    dskipT = din("dskipT", [128, 8])
    bgluT = din("bgluT", [128, 8])
    wglu_bd = din("wglu_bd", [8, 128, 128])
    lnrows = din("lnrows", [4, D])
    w_router = din("w_router", [D, NE])
    b_router = din("b_router", [1, NE])
    if with_moe:
        w_gu = din("w_gu", [KNOB.get('nexp', NE), D, 2 * D])
        w_dn = din("w_dn", [KNOB.get('nexp', NE), D, D])
        b_guT = din("b_guT", [128, NE, 32])
        b_dn = din("b_dn", [NE, D])
    consts = din("consts", [128, 128 + 16 + 8 + 1])

    y_out = dout("y", [TOK, D])
    npool = dout("npool", [2, 15, 1024])
    nssm = dout("nssm", [2, 64, 128])
    dbg = {}
    if debug:
        dbg['uT'] = dout("dbg_uT", [D, TOK])
        dbg['mixinT'] = dout("dbg_mixinT", [D, TOK])
        dbg['x1'] = dout("dbg_x1", [TOK, D])
        dbg['gates'] = dout("dbg_gates", [TOK, NE])
        dbg['modpp'] = dout("dbg_modpp", [128, 128])

    modrow = dscr("modrow", [4, D])
    x1_scr = dscr("x1_scr", [TOK, D])
    h2T_scr = dscr("h2T_scr", [128, 16, TOK], F32)

    P = Prog(nc)
    outs_keys = []

    with ExitStack() as st:
        def sb(name, shape, dt):
            return st.enter_context(nc.sbuf_tensor(name, list(shape), dt))

        AW = 44000
        arena = sb("arena", [128, AW], F32)

        def carve(off, words, dt=F32):
            assert off + words <= AW, (off, words)
            v = arena[:, off:off + words]
            if dt == BF16:
                v = v.bitcast(BF16)
            return v

        pb = [st.enter_context(nc.psum_tensor("pb%d" % i, [128, 512], F32)) for i in range(4)]
        pd = [st.enter_context(nc.psum_tensor("pd%d" % i, [128, 1024], F32)) for i in range(2)]
        pb += [pd[0][:, 0:512], pd[0][:, 512:1024], pd[1][:, 0:512], pd[1][:, 512:1024]]
        cst = sb("cst", [128, 153], F32)
        ident = cst[:, 0:128]
        invc = cst[:, 128:144]
        rowmask = cst[:, 144:152]
        sgn = cst[:, 152:153]
        modpp = sb("modpp", [128, 4, 16, 2], F32)
        gates = sb("gates", [128, 17, NE], F32)
        usave = sb("usave", [128, 8, 32], F32)
        small = sb("small", [128, 64], F32)
        P.dma('sp', lambda e: [e.dma_start(out=cst[:], in_=consts[:, :])], 1, 'cst', writes=['cst'])

        cT_s = sb("cT_s", [128, 16, 2], F32)
        sil = sb("sil", [128, 16, 2], F32)
        sil2 = sb("sil2", [128, 16, 2], BF16)
        badaT = sb("badaT", [128, 96], F32)
        srep = [carve(8192 + w * 1024, 1024, BF16).rearrange("p (k n) -> p k n", k=16) for w in range(2)]
        wada = [carve(b * 4096, 4096, BF16).rearrange("p (k n) -> p k n", k=16) for b in range(2)]
        bbc = [carve(10240 + b * 512, 512) for b in range(2)]
        gstg = [carve(11264 + b * 512, 512) for b in range(2)]
        P.dma('sp', lambda e: [e.dma_start(out=cT_s[:], in_=cT[:, :, :]), e.dma_start(out=badaT[:], in_=b_adaT[:, :])],
              2, 'cT', writes=['cT', 'badaT'])
        P.act(lambda e: e.activation(sil[:], cT_s[:], AF.Sigmoid), reads=['cT'], writes=['sil'])
        P.dve(lambda e: e.tensor_tensor(sil[:], sil[:], cT_s[:], op=ALU.mult), reads=['sil', 'cT'], writes=['sil'])
        P.dve(lambda e: e.tensor_copy(sil2[:], sil[:]), reads=['sil'], writes=['sil2'])
        for w in range(2):
            P.dve((lambda w: lambda e: e.tensor_copy(srep[w], sil[:, :, w:w + 1].to_broadcast([128, 16, 128])))(w),
                  reads=['sil'], writes=['srep%d' % w])
        pp_map = {0: 0, 1: 1, 3: 2, 4: 3}
        row_map = {2: 0, 5: 2}
        for j in range(24):
            sec, q = j // 4, j % 4
            b = j % 2
            P.dma('pool', (lambda j, b: lambda e: [e.dma_start(
                out=wada[b], in_=w_ada[:, j * 512:(j + 1) * 512].rearrange("(k p) c -> p k c", p=128))])(j, b),
                1, 'wada%d' % b, writes=['wada%d' % b])
            if sec in pp_map:
                s4 = pp_map[sec]
                for sub in range(4):
                    col = sec * 16 + q * 4 + sub
                    kdx = q * 4 + sub
                    pbk = 'pb%d' % (sub % 2)
                    P.pe((lambda b, sub: lambda e: [e.matmul(pb[sub % 2][:, 0:2], lhsT=wada[b][:, kd, sub * 128:(sub + 1) * 128],
                                                             rhs=sil2[:, kd, :], start=(kd == 0), stop=(kd == 15))
                                                    for kd in range(16)])(b, sub),
                         reads=['wada%d' % b, 'sil2'], writes=[pbk])
                    one = 1.0 if sec in (1, 4) else 0.0
                    P.dve((lambda s4, kdx, col, sub, one: lambda e: e.tensor_scalar(
                        modpp[:, s4, kdx, :], pb[sub % 2][:, 0:2], badaT[:, col:col + 1], one, op0=ALU.add, op1=ALU.add))(s4, kdx, col, sub, one),
                        reads=[pbk, 'badaT'], writes=['modpp'])
            else:
                P.dma('sp', (lambda j, b: lambda e: [e.dma_start(
                    out=bbc[b], in_=b_ada_row[0:1, j * 512:(j + 1) * 512].to_broadcast([128, 512]))])(j, b),
                    1, 'bbc%d' % b, writes=['bbc%d' % b])
                for w in range(2):
                    pbk = 'pb%d' % (2 + w)
                    P.pe((lambda b, w: lambda e: [e.matmul(pb[2 + w][:, :], lhsT=srep[w][:, kd, :], rhs=wada[b][:, kd, :],
                                                           start=(kd == 0), stop=(kd == 15)) for kd in range(16)])(b, w),
                         reads=['wada%d' % b, 'srep%d' % w], writes=[pbk])
                    P.dve((lambda b, w: lambda e: e.tensor_tensor(gstg[w], pb[2 + w][:, :], bbc[b], op=ALU.add))(b, w),
                          reads=[pbk, 'bbc%d' % b], writes=['gstg%d' % w])
                    ridx = row_map[sec] + w
                    P.dma('sp', (lambda ridx, q, w: lambda e: [e.dma_start(
                        out=modrow[ridx:ridx + 1, q * 512:(q + 1) * 512], in_=gstg[w][0:1, :])])(ridx, q, w),
                        1, 'gstg%d' % w, reads=['gstg%d' % w], writes=['modrow'])
        if debug:
            P.dma('sp', lambda e: [e.dma_start(out=dbg['modpp'][:, :], in_=modpp[:].rearrange("p a k w -> p (a k w)"))],
                  1, 'dbgmod', reads=['modpp'], writes=['dbg_modpp'])
            outs_keys.append('dbg_modpp')

        if stop_after == 'A':
            P.finalize(final_reads=outs_keys)
            return nc
        O_HT = 0
        O_R2 = 16512
        O_UP = 23712
        O_US = 31968
        hT = carve(O_HT, 16512, BF16).rearrange("p (k n) -> p k n", k=16)
        mixinT = hT
        winr = [carve(O_R2 + i * 1024, 1024, BF16).rearrange("p (k n) -> p k n", k=16) for i in range(3)]
        xst = [carve(O_R2 + 3072 + i * 2064, 2064) for i in range(2)]
        uP = carve(O_UP, 8256, BF16).rearrange("p (k n) -> p k n", k=8)
        uS = carve(O_US, 8256, BF16).rearrange("p (k n) -> p k n", k=8)
        P.barrier()
        for kd in range(16):
            b = kd % 2
            P.dma('sp', (lambda kd, b: lambda e: [e.dma_start(out=xst[b], in_=xT[kd * 128:(kd + 1) * 128, :])])(kd, b),
                  1, 'xst%d' % b, writes=['xst%d' % b])
            extra = []
            P.act((lambda kd, b: lambda e: [
                e.activation(hT[:, kd, 0:NP_], xst[b][:, 0:NP_], AF.Identity, bias=modpp[:, 0, kd, 0:1], scale=modpp[:, 1, kd, 0:1]),
                e.activation(hT[:, kd, NP_:TOK], xst[b][:, NP_:TOK], AF.Identity, bias=modpp[:, 0, kd, 1:2], scale=modpp[:, 1, kd, 1:2]),
            ])(kd, b), reads=['xst%d' % b, 'modpp'], writes=['hT'] + extra)
        if stop_after == 'B1':
            P.finalize(final_reads=outs_keys)
            return nc
        ev = 0
        for m in range(KNOB.get('nm', 16)):
            r = m % 3
            P.dma('pool', (lambda m, r: lambda e: [e.dma_start(
                out=winr[r], in_=w_in[:, m * 128:(m + 1) * 128].rearrange("(k p) c -> p k c", p=128))])(m, r),
                1, 'win%d' % r, writes=['win%d' % r])
            for ci, (c0, cn) in enumerate(CHUNKS):
                bk = ev % 2
                pbk = 'pb%d' % bk
                P.pe((lambda r, bk, c0, cn: lambda e: [e.matmul(pb[bk][:, 0:cn], lhsT=winr[r][:, kd, :], rhs=hT[:, kd, c0:c0 + cn],
                                                               start=(kd == 0), stop=(kd == 15)) for kd in range(16)])(r, bk, c0, cn),
                     reads=['win%d' % r, 'hT'], writes=[pbk])
                dst = uP[:, m, c0:c0 + cn] if m < 8 else uS[:, m - 8, c0:c0 + cn]
                dkey = 'uT%d' % m
                if ev % 2 == 0:
                    P.act((lambda dst, bk, cn: lambda e: e.copy(dst, pb[bk][:, 0:cn]))(dst, bk, cn), reads=[pbk], writes=[dkey])
                else:
                    P.dve((lambda dst, bk, cn: lambda e: e.tensor_copy(dst, pb[bk][:, 0:cn]))(dst, bk, cn), reads=[pbk], writes=[dkey])
                if m < 8 and ci == 3 and not KNOB.get('nousave'):
                    P.dve((lambda m, bk: lambda e: e.tensor_copy(usave[:, m, 0:16], pb[bk][:, 496:512]))(m, bk), reads=[pbk, dkey], writes=['usave'])
                if m < 8 and ci == 4 and not KNOB.get('nousave'):
                    P.dve((lambda m, bk: lambda e: e.tensor_copy(usave[:, m, 16:32], pb[bk][:, 0:16]))(m, bk), reads=[pbk, dkey], writes=['usave'])
                ev += 1
        if stop_after == 'B2':
            P.finalize(final_reads=outs_keys)
            return nc
        nps = carve(40224, 1024)
        for m in range(8):
            P.pe((lambda m: lambda e: e.transpose(pb[2 + (m % 2)][0:32, 0:128], usave[:, m, :], ident))(m),
                 reads=['usave', 'cst'], writes=['pb%d' % (2 + (m % 2))])
            P.dve((lambda m: lambda e: e.tensor_copy(nps[0:32, m * 128:(m + 1) * 128], pb[2 + (m % 2)][0:32, 0:128]))(m),
                  reads=['pb%d' % (2 + (m % 2))], writes=['nps'])
        P.dma('sp', lambda e: [e.dma_start(out=npool[0], in_=nps[1:16, :]), e.dma_start(out=npool[1], in_=nps[17:32, :])],
              2, 'npool', reads=['nps'], writes=['npool'])
        outs_keys.append('npool')
        if debug:
            for m in range(16):
                src = uP[:, m, :] if m < 8 else uS[:, m - 8, :]
                P.dve((lambda src: lambda e: e.tensor_copy(xst[0], src))(src), reads=['uT%d' % m], writes=['xst0'])
                P.dma('sp', (lambda m: lambda e: [e.dma_start(out=dbg['uT'][m * 128:(m + 1) * 128, :], in_=xst[0])])(m),
                      1, 'xst0', reads=['xst0'], writes=['dbg_uT%d' % m])
                outs_keys.append('dbg_uT%d' % m)

        if stop_after == 'B':
            P.finalize(final_reads=outs_keys)
            return nc
        E0 = carve(O_R2, 2048)
        E1 = carve(O_R2 + 2048, 2048)
        diffT = carve(O_R2 + 4096, 2064, BF16).rearrange("p (k n) -> p k n", k=2)
        wpl = [carve(O_R2 + 6160 + i * 256, 256, BF16).rearrange("p (k n) -> p k n", k=2) for i in range(2)]
        Es = [sb("Es%d" % i, [128, 31], F32) for i in range(2)]
        pscT = sb("pscT", [128, 8], F32)
        P.dma('sp', lambda e: [e.dma_start(out=pscT[:], in_=pool_scaleT[:, :])], 1, 'pscT', writes=['pscT'])
        B_KEYS = []
        P.barrier()
        first = True
        for g in range(4):
            wdw = POOL_W[g]
            nst = {2: 1, 4: 2, 8: 3, 16: 4}[wdw]
            P.dma('pool', (lambda g: lambda e: [e.dma_start(out=wpl[g % 2], in_=w_pool[g].rearrange("(k p) c -> p k c", p=128))])(g),
                  1, 'wpl%d' % (g % 2), writes=['wpl%d' % (g % 2)] + (B_KEYS if first else []))
            for i in range(2):
                m = 2 * g + i
                uk = 'uT%d' % m
                src = uP[:, m, 0:NP_]
                bufs = [E0, E1]
                cur = None
                k = 1
                for s_ in range(nst):
                    dstb = bufs[s_ % 2]
                    a_in = src if cur is None else cur
                    rk = [uk] if cur is None else ['E%d' % ((s_ + 1) % 2)]
                    P.dve((lambda dstb, a_in, k: lambda e: [
                        e.tensor_tensor(dstb[:, k:NP_], a_in[:, k:NP_], a_in[:, 0:NP_ - k], op=ALU.add),
                        e.tensor_copy(dstb[:, 0:k], a_in[:, 0:k])])(dstb, a_in, k),
                        reads=rk, writes=['E%d' % (s_ % 2)] + (B_KEYS if first else []))
                    first = False
                    cur = dstb
                    k *= 2
                ck = 'E%d' % ((nst - 1) % 2)
                other = bufs[nst % 2]
                ok = 'E%d' % (nst % 2)
                P.dve((lambda cur, src, i, wdw: lambda e: e.scalar_tensor_tensor(
                    diffT[:, i, 0:NP_], in0=cur[:, 0:NP_], scalar=1.0 / wdw, in1=src, op0=ALU.mult, op1=ALU.subtract))(cur, src, i, wdw),
                    reads=[ck, uk], writes=['diffT%d' % i])
                P.dve((lambda cur, other, src, i, wdw: lambda e: [
                    e.tensor_tensor(other[:, 0:wdw - 1], cur[:, 0:wdw - 1], invc[:, 0:wdw - 1], op=ALU.mult),
                    e.tensor_tensor(diffT[:, i, 0:wdw - 1], other[:, 0:wdw - 1], src[:, 0:wdw - 1], op=ALU.subtract)])(cur, other, src, i, wdw),
                    reads=[ck, uk, 'cst', ok], writes=['diffT%d' % i, ok])
                P.dma('sp', (lambda m: lambda e: [e.dma_start(out=Es[0][:, 0:15], in_=cache_poolT[m * 128:(m + 1) * 128, :])])(m),
                      1, 'Es0', writes=['Es0'])
                P.dve((lambda m: lambda e: e.tensor_copy(Es[0][:, 15:31], usave[:, m, 16:32]))(m), reads=['usave'], writes=['Es0'])
                k = 1
                cs = 0
                for s_ in range(nst):
                    P.dve((lambda cs, k: lambda e: e.tensor_tensor(Es[1 - cs][:, 2 * k - 1:31], Es[cs][:, 2 * k - 1:31], Es[cs][:, k - 1:31 - k], op=ALU.add))(cs, k),
                          reads=['Es%d' % cs], writes=['Es%d' % (1 - cs)])
                    cs = 1 - cs
                    k *= 2
                P.dve((lambda cs, m, i, wdw: lambda e: e.scalar_tensor_tensor(
                    diffT[:, i, NP_:TOK], in0=Es[cs][:, 15:31], scalar=1.0 / wdw, in1=usave[:, m, 16:32], op0=ALU.mult, op1=ALU.subtract))(cs, m, i, wdw),
                    reads=['Es%d' % cs, 'usave'], writes=['diffT%d' % i])
            for j in range(2):
                mo = 2 * g + j
                for ci, (c0, cn) in enumerate(CHUNKS):
                    bk = ev % 2
                    pbk = 'pb%d' % bk
                    P.pe((lambda g, j, bk, c0, cn: lambda e: [e.matmul(pb[bk][:, 0:cn], lhsT=wpl[g % 2][:, i, j * 128:(j + 1) * 128],
                                                                      rhs=diffT[:, i, c0:c0 + cn], start=(i == 0), stop=(i == 1)) for i in range(2)])(g, j, bk, c0, cn),
                         reads=['wpl%d' % (g % 2), 'diffT0', 'diffT1'], writes=[pbk])
                    P.act((lambda mo, bk, c0, cn: lambda e: e.activation(mixinT[:, mo, c0:c0 + cn], pb[bk][:, 0:cn], AF.Copy, scale=pscT[:, mo:mo + 1]))(mo, bk, c0, cn),
                          reads=[pbk, 'pscT', 'hT'], writes=['mix%d' % mo])
                    ev += 1

        if stop_after == 'C':
            P.finalize(final_reads=outs_keys)
            return nc
        P.barrier()
        lam = sb("lam", [128, 2, 64], F32)
        ldt = sb("ldt", [128, 64], F32)
        prm = carve(16512 + 12448 + 2192, 768).rearrange("p (a n) -> p a n", a=12)
        MAGt = sb("MAGt", [128, 64], F32)
        THt = sb("THt", [128, 64], F32)
        K1 = sb("K1", [128, 64], F32)
        K2 = sb("K2", [128, 64], F32)
        h0s = sb("h0s", [128, 64], F32)
        dsk = sb("dsk", [128, 8], F32)
        bgl = sb("bgl", [128, 8], F32)
        G1l = sb("G1l", [128, 2, 64], F32)
        G2l = sb("G2l", [128, 2, 64], F32)
        perm = sb("perm", [128, 128], F32)
        halfpi = sb("halfpi", [128, 1], F32)
        P.dma('sp', lambda e: [e.dma_start(out=lam[:], in_=lamst[:, :, :]),
                               e.dma_start(out=ldt[:], in_=logdt_row[0:1, :].to_broadcast([128, 64])),
                               e.dma_start(out=h0s[:], in_=h0st[:, :]),
                               e.dma_start(out=dsk[:], in_=dskipT[:, :]),
                               e.dma_start(out=bgl[:], in_=bgluT[:, :])], 5, 'ssmp', writes=['ssmp'])
        lr, li = lam[:, 0, :], lam[:, 1, :]
        dt_, lrdt, th, fr, cs_, sn_, are, aim, den, t1, t2, t3 = [prm[:, i, :] for i in range(12)]

        def pm():
            def f(e):
                r = []
                r.append(e.memset(halfpi[:], float(np.pi / 2)))
                return r
            return f
        P.pool(lambda e: e.memset(halfpi[:], float(np.pi / 2)), writes=['halfpi'])
        P.act(lambda e: e.activation(dt_, ldt[:], AF.Exp), reads=['ssmp'], writes=['p_dt'])
        P.dve(lambda e: [e.tensor_tensor(lrdt, lr, dt_, op=ALU.mult),
                         e.tensor_tensor(th, li, dt_, op=ALU.mult)], reads=['p_dt', 'ssmp'], writes=['p_a'])
        P.act(lambda e: e.activation(MAGt[:], lrdt, AF.Exp), reads=['p_a'], writes=['MAG'])
        P.dve(lambda e: [e.tensor_scalar(th, th, float(1.0 / (2 * np.pi)), None, op0=ALU.mult),
                         e.tensor_scalar(fr, th, MAGIC, None, op0=ALU.add),
                         e.tensor_scalar(fr, fr, -MAGIC, None, op0=ALU.add),
                         e.tensor_tensor(THt[:], th, fr, op=ALU.subtract)], reads=['p_a'], writes=['TH'])
        P.act(lambda e: [e.activation(sn_, THt[:], AF.Sin, scale=float(2 * np.pi)),
                         e.activation(t1, THt[:], AF.Abs),
                         e.activation(cs_, t1, AF.Sin, bias=halfpi[:, 0:1], scale=float(-2 * np.pi))],
              reads=['TH', 'halfpi'], writes=['p_trig'])
        P.dve(lambda e: [
            e.tensor_tensor(are, MAGt[:], cs_, op=ALU.mult),
            e.tensor_scalar(are, are, -1.0, None, op0=ALU.add),
            e.tensor_tensor(aim, MAGt[:], sn_, op=ALU.mult),
            e.tensor_tensor(den, lr, lr, op=ALU.mult),
            e.tensor_tensor(t1, li, li, op=ALU.mult),
            e.tensor_tensor(den, den, t1, op=ALU.add),
            e.reciprocal(den, den),
            e.tensor_tensor(t1, are, lr, op=ALU.mult),
            e.tensor_tensor(t2, aim, li, op=ALU.mult),
            e.tensor_tensor(t1, t1, t2, op=ALU.add),
            e.tensor_tensor(K1[:], t1, den, op=ALU.mult),
            e.tensor_tensor(t1, aim, lr, op=ALU.mult),
            e.tensor_tensor(t2, are, li, op=ALU.mult),
            e.tensor_tensor(t1, t1, t2, op=ALU.subtract),
            e.tensor_tensor(t3, t1, den, op=ALU.mult),
            e.tensor_scalar(K2[:], t3, sgn[:, 0:1], -1.0, op0=ALU.mult, op1=ALU.mult),
            e.tensor_scalar(perm[:, 0:64], ident[:, 64:128], -1.0, None, op0=ALU.mult),
            e.tensor_copy(perm[:, 64:128], ident[:, 0:64]),
        ], reads=['MAG', 'p_trig', 'ssmp', 'cst'], writes=['Kp', 'perm'])

        O_D = O_R2
        CP = carve(O_D, 2080)
        SP = carve(O_D + 2080, 2080)
        bpr = carve(O_D + 4160, 2064)
        iot = carve(O_D + 6224, 2080)
        yvF = carve(O_D + 8304, 2080)
        yv = yvF[:, 0:TOK]
        yg = carve(O_D + 10384, 2064)
        ang = yvF
        tmpr = yg
        gsc = yg
        O_G = O_D + 12448
        G1b = carve(O_G, 1032, BF16)
        G2b = carve(O_G + 1032, 1032, BF16)
        packs = carve(O_G + 2064, 128)
        O_T = 40224
        BB = carve(O_T, 1024)
        Clb = carve(O_T + 1024, 1024, BF16).rearrange("p (a n) -> p a n", a=2)
        LB = carve(O_T + 2048, 1024, BF16).rearrange("p (g a n) -> p g a n", g=8, a=2)
        LC = sb("LC", [128, 8, 2, 128], BF16)
        c5buf = carve(O_T + 3072, 512)
        ygb = G1b
        Bl = yv[:, 0:2048].rearrange("p (a n) -> p a n", a=2)
        Cl = yg[:, 0:2048].rearrange("p (a n) -> p a n", a=2)
        wgl = sb("wgl", [128, 128], BF16)
        C_KEYS = []
        P.pool(lambda e: e.iota(iot, pattern=[[1, 2080]], base=0, channel_multiplier=0, allow_small_or_imprecise_dtypes=True), writes=['iot'])
        P.dma('sp', lambda e: [e.dma_start(out=Bl[:, 0, :], in_=Bst[0]), e.dma_start(out=Bl[:, 1, :], in_=Bst[1]),
                               e.dma_start(out=Cl[:, 0, :], in_=Cst[0]), e.dma_start(out=Cl[:, 1, :], in_=Cst[1])], 4, 'BC', writes=['BC', 'yv', 'yg'])
        BB3 = BB.rearrange("p (g c) -> p g c", g=64)
        P.dve(lambda e: [
            e.tensor_tensor(BB3, Bl[:, 0, :].rearrange("p (g c) -> p g c", g=64), K1[:].unsqueeze(2).to_broadcast([128, 64, 16]), op=ALU.mult),
            e.tensor_tensor(Bl[:, 1, :].rearrange("p (g c) -> p g c", g=64), Bl[:, 1, :].rearrange("p (g c) -> p g c", g=64),
                            K2[:].unsqueeze(2).to_broadcast([128, 64, 16]), op=ALU.mult),
            e.tensor_tensor(BB, BB, Bl[:, 1, :], op=ALU.add),
            e.tensor_scalar(Clb[:, 0, :], Cl[:, 0, :], sgn[:, 0:1], None, op0=ALU.mult),
            e.tensor_scalar(Clb[:, 1, :], Cl[:, 1, :], -1.0, None, op0=ALU.mult),
        ], reads=['BC', 'Kp', 'cst', 'yv', 'yg'], writes=['BBC', 'yv', 'yg'])
        P.pool(lambda e: e.memset(LC[:], 0.0), writes=['LC'])
        for blk in range(8):
            ukey = 'uT%d' % (8 + blk)
            ut = uS[:, blk, :]
            P.pe((lambda blk: lambda e: e.transpose(pb[7][:, 0:128], BB[:, blk * 128:(blk + 1) * 128], ident))(blk),
                 reads=['BBC', 'cst'], writes=['pb7'])
            P.act(lambda e: e.copy(packs, pb[7][:, 0:128]), reads=['pb7'], writes=['packs'])

            def lbprep(blk):
                def f(e):
                    r = []
                    for gq in range(8):
                        r.append(e.tensor_scalar(LB[:, gq, 0, :], packs, rowmask[:, gq:gq + 1], None, op0=ALU.mult))
                        r.append(e.tensor_scalar(LB[:, gq, 1, 0:64], packs[:, 64:128], rowmask[:, gq:gq + 1], None, op0=ALU.mult))
                        r.append(e.tensor_scalar(LB[:, gq, 1, 64:128], packs[:, 0:64], rowmask[:, gq:gq + 1], -1.0, op0=ALU.mult, op1=ALU.mult))
                        g = blk * 8 + gq
                        r.append(e.tensor_copy(LC[:, gq, 0, gq * 16:(gq + 1) * 16], Clb[:, 0, g * 16:(g + 1) * 16]))
                        r.append(e.tensor_copy(LC[:, gq, 1, gq * 16:(gq + 1) * 16], Clb[:, 1, g * 16:(g + 1) * 16]))
                    return r
                return f
            P.dve(lbprep(blk), reads=['packs', 'BBC', 'cst'], writes=['LB', 'LC'], indep=True)
            for gq in range(8):
                g = blk * 8 + gq
                def ang_pool(g):
                    P.pool((lambda g: lambda e: [
                        e.tensor_scalar(ang, iot, THt[:, g:g + 1], MAGIC, op0=ALU.mult, op1=ALU.add),
                        e.tensor_scalar(ang, ang, -MAGIC, None, op0=ALU.add)])(g),
                        reads=['iot', 'TH'], writes=['yv'])

                def ang_dve(g):
                    P.dve((lambda g: lambda e: e.scalar_tensor_tensor(ang, in0=iot, scalar=THt[:, g:g + 1], in1=ang, op0=ALU.mult, op1=ALU.subtract))(g),
                          reads=['iot', 'TH', 'yv'], writes=['yv'])
                if gq == 0:
                    ang_pool(g)
                    ang_dve(g)
                P.act(lambda e: [e.activation(SP, ang, AF.Sin, scale=float(2 * np.pi)),
                                 e.activation(ang, ang, AF.Abs),
                                 e.activation(CP, ang, AF.Sin, bias=halfpi[:, 0:1], scale=float(-2 * np.pi))],
                      reads=['yv', 'halfpi'], writes=['CP', 'SP', 'yv'])
                if gq < 7:
                    ang_pool(g + 1)
                for ci, (c0, cn) in enumerate(CHUNKS):
                    t0 = c0 if ci < 4 else 1
                    P.pe((lambda ut, gq, c0, cn: lambda e: [
                        e.matmul(pb[5][:, 0:cn], lhsT=LB[:, gq, 0, :], rhs=ut[:, c0:c0 + cn], start=True, stop=True),
                        e.matmul(pb[6][:, 0:cn], lhsT=LB[:, gq, 1, :], rhs=ut[:, c0:c0 + cn], start=True, stop=True)])(ut, gq, c0, cn),
                        reads=['LB', ukey], writes=['pb5', 'pb6'])
                    P.dve((lambda c0, cn, t0: lambda e: [
                        e.tensor_tensor(tmpr[:, c0:c0 + cn], pb[5][:, 0:cn], CP[:, t0:t0 + cn], op=ALU.mult),
                        e.tensor_tensor(bpr[:, c0:c0 + cn], pb[6][:, 0:cn], SP[:, t0:t0 + cn], op=ALU.mult)])(c0, cn, t0),
                        reads=['pb5', 'pb6', 'CP', 'SP'], writes=['bpr', 'yg'], indep=True)
                    P.dve((lambda c0, cn: lambda e: e.tensor_tensor(bpr[:, c0:c0 + cn], bpr[:, c0:c0 + cn], tmpr[:, c0:c0 + cn], op=ALU.add))(c0, cn),
                          reads=['bpr', 'yg'], writes=['bpr'])
                if gq < 7:
                    ang_dve(g + 1)
                P.dve((lambda g: lambda e: [
                    e.tensor_tensor_scan(gsc[:, 0:NP_], MAGt[:, g:g + 1].to_broadcast([128, NP_]), bpr[:, 0:NP_], 0.0, op0=ALU.mult, op1=ALU.add),
                    e.tensor_tensor_scan(gsc[:, NP_:TOK], MAGt[:, g:g + 1].to_broadcast([128, NS_]), bpr[:, NP_:TOK], h0s[:, g:g + 1], op0=ALU.mult, op1=ALU.add)])(g),
                    reads=['bpr', 'MAG', 'ssmp'], writes=['yg'], indep=True)
                P.dve((lambda g: lambda e: [
                    e.tensor_tensor(G1b[:, 0:NP_], gsc[:, 0:NP_], CP[:, 0:NP_], op=ALU.mult),
                    e.tensor_tensor(G1b[:, NP_:TOK], gsc[:, NP_:TOK], CP[:, 1:1 + NS_], op=ALU.mult),
                    e.tensor_tensor(G1l[:, 0, g:g + 1], gsc[:, NP_ - 1:NP_], CP[:, NP_ - 1:NP_], op=ALU.mult),
                    e.tensor_tensor(G1l[:, 1, g:g + 1], gsc[:, TOK - 1:TOK], CP[:, NS_:NS_ + 1], op=ALU.mult),
                    e.tensor_tensor(G2l[:, 0, g:g + 1], gsc[:, NP_ - 1:NP_], SP[:, NP_ - 1:NP_], op=ALU.mult),
                    e.tensor_tensor(G2l[:, 1, g:g + 1], gsc[:, TOK - 1:TOK], SP[:, NS_:NS_ + 1], op=ALU.mult)])(g),
                    reads=['yg', 'CP', 'SP'], writes=['G1b', 'Gl'], indep=True)
                P.pool(lambda e: [
                    e.tensor_tensor(G2b[:, 0:NP_], gsc[:, 0:NP_], SP[:, 0:NP_], op=ALU.mult),
                    e.tensor_tensor(G2b[:, NP_:TOK], gsc[:, NP_:TOK], SP[:, 1:1 + NS_], op=ALU.mult)],
                    reads=['yg', 'SP'], writes=['G2b'], indep=True)
                for ci, (c0, cn) in enumerate(CHUNKS):
                    P.pe((lambda gq, ci, c0, cn: lambda e: [
                        e.matmul(pb[ci][:, 0:cn], lhsT=LC[:, gq, 0, :], rhs=G1b[:, c0:c0 + cn], start=(gq == 0), stop=False),
                        e.matmul(pb[ci][:, 0:cn], lhsT=LC[:, gq, 1, :], rhs=G2b[:, c0:c0 + cn], start=False, stop=(gq == 7))])(gq, ci, c0, cn),
                        reads=['LC', 'G1b', 'G2b'], writes=['pb%d' % ci])
            P.dma('pool', (lambda blk: lambda e: [e.dma_start(out=wgl[:], in_=wglu_bd[blk])])(blk), 1, 'wgl', writes=['wgl'])
            for ci, (c0, cn) in enumerate(CHUNKS):
                P.dve((lambda ut, blk, ci, c0, cn: lambda e: e.scalar_tensor_tensor(
                    yv[:, c0:c0 + cn], in0=ut[:, c0:c0 + cn], scalar=dsk[:, blk:blk + 1], in1=pb[ci][:, 0:cn], op0=ALU.mult, op1=ALU.add))(ut, blk, ci, c0, cn),
                    reads=['pb%d' % ci, ukey, 'ssmp'], writes=['yv'])
            P.dve(lambda e: [
                e.tensor_tensor(yg, yv, yv, op=ALU.mult),
                e.tensor_scalar(yg, yg, 0.044715, 1.0, op0=ALU.mult, op1=ALU.add),
                e.tensor_tensor(yg, yg, yv, op=ALU.mult)], reads=['yv'], writes=['yg'])
            P.act(lambda e: e.activation(yg, yg, AF.Sigmoid, scale=1.5957691216057308), reads=['yg'], writes=['yg'])
            P.dve(lambda e: [e.tensor_tensor(yg, yg, yv, op=ALU.mult),
                             e.tensor_copy(ygb, yg)], reads=['yg', 'yv'], writes=['yg', 'G1b'])
            for ci, (c0, cn) in enumerate(CHUNKS):
                bk = 5 + (ci % 2)
                P.pe((lambda bk, c0, cn: lambda e: e.matmul(pb[bk][:, 0:cn], lhsT=wgl[:], rhs=ygb[:, c0:c0 + cn], start=True, stop=True))(bk, c0, cn),
                     reads=['wgl', 'G1b'], writes=['pb%d' % bk])
                P.act((lambda blk, bk, c0, cn: lambda e: e.activation(yv[:, c0:c0 + cn], pb[bk][:, 0:cn], AF.Sigmoid, bias=bgl[:, blk:blk + 1]))(blk, bk, c0, cn),
                      reads=['pb%d' % bk, 'ssmp'], writes=['yv'])
                P.dve((lambda blk, c0, cn: lambda e: e.tensor_tensor(mixinT[:, 8 + blk, c0:c0 + cn], yg[:, c0:c0 + cn], yv[:, c0:c0 + cn], op=ALU.mult))(blk, c0, cn),
                      reads=['yv', 'yg', 'hT'], writes=['mix%d' % (8 + blk)])
        nst_s = sb("nst_s", [64, 2, 128], F32)
        for w in range(2):
            P.pe((lambda w: lambda e: [e.matmul(pb[5 + w][0:64, 0:128], lhsT=G1l[:, w, :], rhs=ident, start=True, stop=False),
                                       e.matmul(pb[5 + w][0:64, 0:128], lhsT=G2l[:, w, :], rhs=perm[:], start=False, stop=True)])(w),
                 reads=['Gl', 'perm', 'cst'], writes=['pb%d' % (5 + w)])
            P.act((lambda w: lambda e: e.copy(nst_s[:, w, :], pb[5 + w][0:64, 0:128]))(w), reads=['pb%d' % (5 + w)], writes=['nst%d' % w])
            P.dma('sp', (lambda w: lambda e: [e.dma_start(out=nssm[w], in_=nst_s[:, w, :])])(w), 1, 'nst%d' % w, reads=['nst%d' % w], writes=['nssm%d' % w])
            outs_keys.append('nssm%d' % w)
        if debug:
            for m in range(16):
                P.dve((lambda m: lambda e: e.tensor_copy(yv, mixinT[:, m, :]))(m), reads=['mix%d' % m], writes=['yv'])
                P.dma('sp', (lambda m: lambda e: [e.dma_start(out=dbg['mixinT'][m * 128:(m + 1) * 128, :], in_=yv)])(m),
                      1, 'dbgyv', reads=['yv'], writes=['dbg_mix%d' % m])
                outs_keys.append('dbg_mix%d' % m)

        if stop_after == 'D':
            P.finalize(final_reads=outs_keys)
            return nc
        MIX_KEYS = ['mix%d' % m for m in range(16)]
        D_KEYS = ['CP', 'SP', 'bpr', 'tmpr', 'gsc', 'G1b', 'G2b', 'ang'] + ['uT%d' % m for m in range(8, 16)]
        P.barrier()
        O_E = O_R2
        wout = carve(O_E, 16384, BF16).rearrange("p (k n) -> p k n", k=16)
        O_E2 = O_E + 16384
        g1b = [carve(O_E2, 2048), carve(O_E2, 2048)]
        lng = carve(O_E2 + 2048, 2048)
        lnb = carve(O_E2 + 4096, 2048)
        x1t = carve(O_E2 + 6144, 2048)
        h2fl = carve(O_E2 + 8192, 2048)
        h2f = h2fl.rearrange("p (k n) -> p k n", k=16)
        wr_s = carve(O_E2 + 10240, 512).rearrange("p (k n) -> p k n", k=16)
        brt = sb("brt", [128, NE], F32)
        stats = sb("stats", [128, 4, 6], F32)
        mv = sb("mv", [128, 2], F32)
        top8 = sb("top8", [128, 8], F32)
        lg = sb("lg", [128, NE], F32)
        for q in range(4):
            P.dma('pool', (lambda q: lambda e: [e.dma_start(out=wout[:, :, q * 512:(q + 1) * 512],
                                                            in_=w_out[:, q * 512:(q + 1) * 512].rearrange("(k p) c -> p k c", p=128))])(q),
                  1, 'wout', writes=['wout'])
        P.dma('sp', lambda e: [e.dma_start(out=g1b[0], in_=modrow[0:1, :].to_broadcast([128, D])),
                               e.dma_start(out=lng, in_=lnrows[0:1, :].to_broadcast([128, D])),
                               e.dma_start(out=lnb, in_=lnrows[1:2, :].to_broadcast([128, D])),
                               e.dma_start(out=wr_s[:], in_=w_router.rearrange("(k p) n -> p k n", p=128)),
                               e.dma_start(out=brt[:], in_=b_router[0:1, :].to_broadcast([128, NE]))],
              5, 'Eld', reads=['modrow'], writes=['Eld', 'g1b'])
        for ti, (r0, rn) in enumerate(TT):
            w = 0 if ti < 16 else 1
            if ti == 16:
                P.dma('sp', lambda e: [e.dma_start(out=g1b[0], in_=modrow[1:2, :].to_broadcast([128, D]))], 1, 'g1b2', reads=['modrow'], writes=['g1b'])
            P.dma('sp', (lambda r0, rn: lambda e: [e.dma_start(out=x1t[0:rn, :], in_=xtok[r0:r0 + rn, :])])(r0, rn),
                  1, 'xt_s', writes=['x1t'])
            for q in range(4):
                P.pe((lambda q, r0, rn: lambda e: [e.matmul(pb[q][0:rn, :], lhsT=mixinT[:, m, r0:r0 + rn], rhs=wout[:, m, q * 512:(q + 1) * 512],
                                                            start=(m == 0), stop=(m == 15)) for m in range(16)])(q, r0, rn),
                     reads=MIX_KEYS + ['wout'], writes=['pb%d' % q])
                P.dve((lambda q, rn, w: lambda e: e.tensor_tensor(h2fl[0:rn, q * 512:(q + 1) * 512], pb[q][0:rn, :], g1b[w][0:rn, q * 512:(q + 1) * 512], op=ALU.mult))(q, rn, w),
                      reads=['pb%d' % q, 'Eld', 'g1b'], writes=['h2f0', 'h2f1', 'h2f2', 'h2f3'])
            P.dve((lambda rn: lambda e: [
                e.scalar_tensor_tensor(x1t[0:rn, :], in0=x1t[0:rn, :], scalar=float(DN_ALPHA), in1=h2fl[0:rn, :], op0=ALU.mult, op1=ALU.add),
            ] + [e.bn_stats(stats[0:rn, q, :], x1t[0:rn, q * 512:(q + 1) * 512]) for q in range(4)] + [
                e.bn_aggr(mv[0:rn, :], stats[0:rn, :, :]),
                e.tensor_scalar(mv[0:rn, 1:2], mv[0:rn, 1:2], float(LN_EPS), None, op0=ALU.add)])(rn),
                reads=['x1t', 'h2f0', 'h2f1', 'h2f2', 'h2f3', 'Eld'], writes=['x1t', 'stats'])
            P.act((lambda rn: lambda e: e.activation(mv[0:rn, 1:2], mv[0:rn, 1:2], AF.Sqrt))(rn), reads=['stats'], writes=['stats'])
            P.dve((lambda rn: lambda e: [
                e.reciprocal(mv[0:rn, 1:2], mv[0:rn, 1:2]),
                e.tensor_scalar(x1t[0:rn, :], x1t[0:rn, :], mv[0:rn, 0:1], mv[0:rn, 1:2], op0=ALU.subtract, op1=ALU.mult),
                e.tensor_tensor(x1t[0:rn, :], x1t[0:rn, :], lng[0:rn, :], op=ALU.mult),
                e.tensor_tensor(x1t[0:rn, :], x1t[0:rn, :], lnb[0:rn, :], op=ALU.add)])(rn),
                reads=['x1t', 'xt_s', 'Eld'], writes=['x1t', 'stats'])
            P.dma('sp', (lambda r0, rn: lambda e: [e.dma_start(out=x1_scr[r0:r0 + rn, :], in_=x1t[0:rn, :])])(r0, rn),
                  1, 'x1st', reads=['x1t'], writes=['x1scr%d' % ti])
            if debug:
                P.dma('sp', (lambda r0, rn: lambda e: [e.dma_start(out=dbg['x1'][r0:r0 + rn, :], in_=x1t[0:rn, :])])(r0, rn),
                      1, 'x1dbg', reads=['x1t'], writes=['dbg_x1%d' % ti])
                outs_keys.append('dbg_x1%d' % ti)
            for kq in range(4):
                bk = 4 + (kq % 2)
                P.pe((lambda kq, bk, rn: lambda e: [e.transpose(pb[bk][:, j * 128:j * 128 + rn], x1t[0:rn, (kq * 4 + j) * 128:(kq * 4 + j + 1) * 128], ident[0:rn, 0:rn])
                                                    for j in range(4)])(kq, bk, rn),
                     reads=['x1t', 'cst'], writes=['pb%d' % bk])
                P.act((lambda kq, bk, rn, w: lambda e: [e.activation(h2f[:, kq * 4 + j, 0:rn], pb[bk][:, j * 128:j * 128 + rn], AF.Identity,
                                                                      bias=modpp[:, 2, kq * 4 + j, w:w + 1], scale=modpp[:, 3, kq * 4 + j, w:w + 1])
                                                        for j in range(4)])(kq, bk, rn, w),
                      reads=['pb%d' % bk, 'modpp'], writes=['h2f%d' % kq])
            P.dma('sp', (lambda r0, rn: lambda e: [e.dma_start(out=h2T_scr[:, :, r0:r0 + rn], in_=h2f[:, :, 0:rn])])(r0, rn),
                  1, 'h2st', reads=['h2f%d' % k for k in range(4)], writes=['h2scr%d' % ti])
            P.pe((lambda rn: lambda e: [e.matmul(pb[6][0:rn, 0:NE], lhsT=h2f[:, kd, 0:rn], rhs=wr_s[:, kd, :], start=(kd == 0), stop=(kd == 15)) for kd in range(16)])(rn),
                 reads=['h2f%d' % k for k in range(4)] + ['Eld'], writes=['pb6'])
            P.dve((lambda ti, rn: lambda e: [
                e.tensor_tensor(lg[0:rn, :], pb[6][0:rn, 0:NE], brt[0:rn, :], op=ALU.add),
                e.max(top8[0:rn, :], lg[0:rn, :]),
                e.tensor_scalar(gates[0:rn, ti, :], lg[0:rn, :], top8[0:rn, 3:4], None, op0=ALU.is_ge),
                e.tensor_scalar(lg[0:rn, :], lg[0:rn, :], top8[0:rn, 0:1], None, op0=ALU.subtract)])(ti, rn),
                reads=['pb6', 'Eld'], writes=['lg', 'gates'])
            P.act((lambda rn: lambda e: e.activation(lg[0:rn, :], lg[0:rn, :], AF.Exp))(rn), reads=['lg'], writes=['lg'])
            P.dve((lambda ti, rn: lambda e: [
                e.tensor_tensor(gates[0:rn, ti, :], gates[0:rn, ti, :], lg[0:rn, :], op=ALU.mult),
                e.reduce_sum(mv[0:rn, 0:1], gates[0:rn, ti, :], axis=mybir.AxisListType.X),
                e.reciprocal(mv[0:rn, 0:1], mv[0:rn, 0:1]),
                e.tensor_scalar(gates[0:rn, ti, :], gates[0:rn, ti, :], mv[0:rn, 0:1], None, op0=ALU.mult)])(ti, rn),
                reads=['lg', 'gates', 'stats'], writes=['gates', 'stats'])
            if debug:
                P.dma('sp', (lambda ti, r0, rn: lambda e: [e.dma_start(out=dbg['gates'][r0:r0 + rn, :], in_=gates[0:rn, ti, :])])(ti, r0, rn),
                      1, 'gdbg', reads=['gates'], writes=['dbg_g%d' % ti])
                outs_keys.append('dbg_g%d' % ti)
        if with_moe:
            moe_phase(locals())
        P.finalize(final_reads=outs_keys)
    return nc


def moe_phase(L):
    P = L['P']; pb = L['pb']; pd = L['pd']; carve = L['carve']; gates = L['gates']; ident = L['ident']
    w_gu = L['w_gu']; w_dn = L['w_dn']; b_guT = L['b_guT']; b_dn = L['b_dn']
    h2T_scr = L['h2T_scr']; x1_scr = L['x1_scr']; modrow = L['modrow']; lnrows = L['lnrows']
    y_out = L['y_out']; outs_keys = L['outs_keys']; stats = L['stats']; mv = L['mv']
    NEXP = KNOB.get('nexp', NE)
    P.barrier()
    h2Th = carve(0, 8320, BF16).rearrange("p (k n) -> p k n", k=16)
    acc = carve(8320, 18432).rearrange("p (t n) -> p t n", t=9)
    actT = [carve(26752 + i * 520, 520, BF16) for i in range(4)]
    wgu = [carve(28832 + i * 2048, 2048, BF16).rearrange("p (k n) -> p k n", k=16) for i in range(3)]
    wdn = [carve(34976 + i * 1024, 1024, BF16) for i in range(4)]
    gcs = [carve(39072 + i * 512, 512) for i in range(2)]
    sgs = [carve(40096 + i * 512, 512) for i in range(2)]
    u1s = [carve(41120, 512), carve(41120, 512)]
    evts = [carve(41632, 1024), carve(41632, 1024)]
    bgu = carve(42656, 1024).rearrange("p (e j) -> p e j", e=NE)
    FB = 26752
    x1f = carve(FB, 2048)
    g2b = carve(FB + 2048, 2048)
    l2g = carve(FB + 4096, 2048)
    l2b = carve(FB + 6144, 2048)
    bdn = carve(FB + 8192, 2048)
    gT = carve(FB + 10240, 128)
    P.dma('sp', lambda e: [e.dma_start(out=bgu, in_=b_guT[:, :, :])], 1, 'bgu', writes=['bgu'])
    P.dve(lambda e: e.tensor_scalar(bgu[:, :, 16:32], bgu[:, :, 16:32], 1.0, None, op0=ALU.add), reads=['bgu'], writes=['bgu'])
    for hf in range(2):
        T0 = hf * 1024
        TH = 1024 if hf == 0 else 1040
        chunks_h = [(0, 512), (512, 512)] + ([(1024, 16)] if hf else [])
        tiles_h = [(i * 128, 128) for i in range(8)] + ([(1024, 16)] if hf else [])
        P.barrier()
        P.dma('pool', (lambda T0, TH: lambda e: [e.dma_start(out=h2Th[:, 4 * i:4 * i + 4, 0:TH], in_=h2T_scr[:, 4 * i:4 * i + 4, T0:T0 + TH]) for i in range(4)])(T0, TH),
              4, 'h2Th', writes=['h2Th'])
        P.pool(lambda e: e.memset(acc, 0.0), writes=['acc%d_%d' % (t, h) for t in range(9) for h in range(2)])
        steps = [(ex, f) for ex in range(NEXP) for f in range(16)]
        cnt = {'ev': 0, 'dv': 0}

        def prefetch_gu(si):
            ex, f = steps[si]
            sl = si % 3
            P.dma('pool', (lambda ex, f, sl: lambda e: [
                e.dma_start(out=wgu[sl][:, :, 0:128], in_=w_gu[ex][:, f * 128:(f + 1) * 128].rearrange("(k p) c -> p k c", p=128)),
                e.dma_start(out=wgu[sl][:, :, 128:256], in_=w_gu[ex][:, D + f * 128:D + (f + 1) * 128].rearrange("(k p) c -> p k c", p=128))])(ex, f, sl),
                2, 'wgu%d' % sl, writes=['wgu%d' % sl])

        def prefetch_dn(si):
            ex, f = steps[si]
            sl = si % 4
            P.dma('pool', (lambda ex, f, sl: lambda e: [e.dma_start(out=wdn[sl], in_=w_dn[ex][f * 128:(f + 1) * 128, :])])(ex, f, sl),
                  1, 'wdn%d' % sl, writes=['wdn%d' % sl])

        def gu_parts(si):
            ex, f = steps[si]
            sl = si % 3
            ab = si % 4
            parts = []
            for (c0, cn) in chunks_h:
                c2 = cnt['ev'] % 2
                pg, pu = (0, 1) if c2 == 0 else (2, 3)
                cnt['ev'] += 1
                gc, sg, u1 = gcs[c2], sgs[c2], u1s[c2]

                def mk(bank, col0, k0, k1, sl=sl, c0=c0, cn=cn):
                    def em():
                        P.pe(lambda e: [e.matmul(pb[bank][:, 0:cn], lhsT=wgu[sl][:, kd, col0:col0 + 128], rhs=h2Th[:, kd, c0:c0 + cn],
                                                 start=(kd == 0), stop=(kd == 15)) for kd in range(k0, k1)],
                             reads=['wgu%d' % sl, 'h2Th'], writes=['pb%d' % bank])
                    return em

                def mk_act(ex=ex, f=f, pg=pg, pu=pu, c0=c0, cn=cn, c2=c2, gc=gc, sg=sg, u1=u1, ab=ab):
                    def em():
                        P.dve(lambda e: e.tensor_scalar(gc[:, 0:cn], pb[pg][:, 0:cn], bgu[:, ex, f:f + 1], 7.0, op0=ALU.add, op1=ALU.min),
                              reads=['pb%d' % pg, 'bgu'], writes=['gc%d' % c2])
                        P.act(lambda e: e.activation(sg[:, 0:cn], gc[:, 0:cn], AF.Sigmoid, scale=1.702), reads=['gc%d' % c2], writes=['sg%d' % c2])
                        P.dve(lambda e: e.tensor_scalar(u1[:, 0:cn], pb[pu][:, 0:cn], bgu[:, ex, 16 + f:17 + f], 8.0, op0=ALU.add, op1=ALU.min),
                              reads=['pb%d' % pu, 'bgu'], writes=['u1'])
                        (P.pool if KNOB.get('tt_pool', True) else P.dve)(lambda e: e.tensor_tensor(sg[:, 0:cn], gc[:, 0:cn], sg[:, 0:cn], op=ALU.mult),
                              reads=['gc%d' % c2, 'sg%d' % c2], writes=['sg%d' % c2])
                        P.dve(lambda e: e.scalar_tensor_tensor(actT[ab][:, c0:c0 + cn], in0=u1[:, 0:cn], scalar=-6.0, in1=sg[:, 0:cn], op0=ALU.max, op1=ALU.mult),
                              reads=['u1', 'sg%d' % c2], writes=['actT%d' % ab])
                    return em
                if cn >= 512:
                    subs = [mk(pg, 0, 0, 8), mk(pg, 0, 8, 16), mk(pu, 128, 0, 8), mk(pu, 128, 8, 16)]
                else:
                    subs = [mk(pg, 0, 0, 16), mk(pu, 128, 0, 16)]
                act_em = mk_act()
                last = subs[-1]
                subs[-1] = (lambda last=last, act_em=act_em: (last(), act_em()))
                parts += subs
            return parts

        def down_parts(s0):
            ex, f = steps[s0]
            assert steps[s0 + 1][0] == ex
            parts = []
            order = [0, 6, 1, 2, 3, 7, 4, 5] + ([8] if len(tiles_h) > 8 else [])
            glist = []
            for tl in order:
                glist.append((tl, 0))
            for tl in order:
                glist.append((tl, 1))
            for (tl, hq) in glist:
                r0l, rn = tiles_h[tl]
                ti = hf * 8 + tl

                def em(tl=tl, r0l=r0l, rn=rn, ti=ti, hq=hq, ex=ex, s0=s0):
                    g = cnt['dv'] % 2
                    cnt['dv'] += 1
                    P.pe(lambda e: [e.matmul(pd[g][0:rn, j * 512:(j + 1) * 512], lhsT=actT[(s0 + d) % 4][:, r0l:r0l + rn],
                                             rhs=wdn[(s0 + d) % 4][:, (2 * hq + j) * 512:(2 * hq + j + 1) * 512], start=(d == 0), stop=(d == 1))
                                    for j in range(2) for d in range(2)],
                         reads=['actT%d' % (s0 % 4), 'actT%d' % ((s0 + 1) % 4), 'wdn%d' % (s0 % 4), 'wdn%d' % ((s0 + 1) % 4)], writes=['pd%d' % g])
                    asl = acc[0:rn, tl, hq * 1024:(hq + 1) * 1024]
                    if tl in (6, 7) and not KNOB.get('no_offload'):
                        P.act(lambda e: e.activation(evts[0][0:rn, :], pd[g][0:rn, :], AF.Copy, scale=gates[0:rn, ti, ex:ex + 1]),
                              reads=['pd%d' % g, 'gates'], writes=['evt'])
                        P.pool(lambda e: e.tensor_tensor(asl, asl, evts[0][0:rn, :], op=ALU.add), reads=['evt'], writes=['acc%d_%d' % (tl, hq)])
                    else:
                        P.dve(lambda e: e.scalar_tensor_tensor(asl, in0=pd[g][0:rn, :], scalar=gates[0:rn, ti, ex:ex + 1], in1=asl, op0=ALU.mult, op1=ALU.add),
                              reads=['pd%d' % g, 'gates'], writes=['acc%d_%d' % (tl, hq)])
                parts.append(em)
            return parts

        cnt['eb'] = 0
        assert len(steps) % 2 == 0
        for s0 in range(min(2, len(steps))):
            prefetch_gu(s0)
        for s0 in range(min(4, len(steps))):
            prefetch_dn(s0)
        pending = []
        quota = 0
        for si in range(len(steps)):
            if si + 2 < len(steps):
                prefetch_gu(si + 2)
            gp = gu_parts(si)
            if si % 2 == 0:
                quota = -(-len(pending) // 2)
            else:
                quota = len(pending)
            per = -(-quota // len(gp)) if quota else 0
            done = 0
            for part in gp:
                part()
                for _ in range(per):
                    if pending and done < quota:
                        pending.pop(0)()
                        done += 1
            while pending and done < quota:
                pending.pop(0)()
                done += 1
            if si % 2 == 1:
                assert not pending
                for sn in (si + 1, si + 2):
                    if 4 <= sn < len(steps):
                        prefetch_dn(sn)
                pending = down_parts(si - 1)
        while pending:
            pending.pop(0)()
        P.barrier()
        P.dma('sp', lambda e: [e.dma_start(out=g2b, in_=modrow[2:3, :].to_broadcast([128, D])),
                               e.dma_start(out=l2g, in_=lnrows[2:3, :].to_broadcast([128, D])),
                               e.dma_start(out=l2b, in_=lnrows[3:4, :].to_broadcast([128, D])),
                               e.dma_start(out=bdn[0:NE, :], in_=b_dn[:, :])], 4, 'fin', writes=['fin', 'g2b'])
        for tl, (r0l, rn) in enumerate(tiles_h):
            ti = hf * 8 + tl
            r0 = T0 + r0l
            if ti == 16:
                P.dma('sp', lambda e: [e.dma_start(out=g2b, in_=modrow[3:4, :].to_broadcast([128, D]))], 1, 'fin2', writes=['g2b'])
            P.dma('sp', (lambda r0, rn: lambda e: [e.dma_start(out=x1f[0:rn, :], in_=x1_scr[r0:r0 + rn, :])])(r0, rn), 1, 'x1f', writes=['x1f'])
            P.pe((lambda ti, rn: lambda e: e.transpose(pb[0][0:NE, 0:rn], gates[0:rn, ti, :], ident[0:rn, 0:rn]))(ti, rn), reads=['gates', 'cst'], writes=['pb0'])
            P.act((lambda rn: lambda e: e.copy(gT[0:NE, 0:rn], pb[0][0:NE, 0:rn]))(rn), reads=['pb0'], writes=['gT'])
            for q in range(4):
                P.pe((lambda rn, q: lambda e: e.matmul(pb[4 + q][0:rn, :], lhsT=gT[0:NE, 0:rn], rhs=bdn[0:NE, q * 512:(q + 1) * 512], start=True, stop=True))(rn, q),
                     reads=['gT', 'fin'], writes=['pb%d' % (4 + q)])
                P.dve((lambda tl, rn, q: lambda e: [
                    e.tensor_tensor(acc[0:rn, tl, q * 512:(q + 1) * 512], acc[0:rn, tl, q * 512:(q + 1) * 512], pb[4 + q][0:rn, :], op=ALU.add)])(tl, rn, q),
                    reads=['pb%d' % (4 + q)], writes=['acc%d_%d' % (tl, q // 2)])
            P.dve((lambda tl, rn: lambda e: [
                e.tensor_tensor(acc[0:rn, tl, :], acc[0:rn, tl, :], g2b[0:rn, :], op=ALU.mult),
                e.scalar_tensor_tensor(x1f[0:rn, :], in0=x1f[0:rn, :], scalar=float(DN_ALPHA), in1=acc[0:rn, tl, :], op0=ALU.mult, op1=ALU.add),
            ] + [e.bn_stats(stats[0:rn, q, :], x1f[0:rn, q * 512:(q + 1) * 512]) for q in range(4)] + [
                e.bn_aggr(mv[0:rn, :], stats[0:rn, :, :]),
                e.tensor_scalar(mv[0:rn, 1:2], mv[0:rn, 1:2], float(LN_EPS), None, op0=ALU.add)])(tl, rn),
                reads=['x1f', 'g2b', 'fin'], writes=['x1f', 'acc%d_0' % tl, 'acc%d_1' % tl, 'stats'])
            P.act((lambda rn: lambda e: e.activation(mv[0:rn, 1:2], mv[0:rn, 1:2], AF.Sqrt))(rn), reads=['stats'], writes=['stats'])
            P.dve((lambda rn: lambda e: [
                e.reciprocal(mv[0:rn, 1:2], mv[0:rn, 1:2]),
                e.tensor_scalar(x1f[0:rn, :], x1f[0:rn, :], mv[0:rn, 0:1], mv[0:rn, 1:2], op0=ALU.subtract, op1=ALU.mult),
                e.tensor_tensor(x1f[0:rn, :], x1f[0:rn, :], l2g[0:rn, :], op=ALU.mult),
                e.tensor_tensor(x1f[0:rn, :], x1f[0:rn, :], l2b[0:rn, :], op=ALU.add)])(rn),
                reads=['stats', 'x1f', 'fin'], writes=['x1f', 'stats'])
            P.dma('sp', (lambda r0, rn: lambda e: [e.dma_start(out=y_out[r0:r0 + rn, :], in_=x1f[0:rn, :])])(r0, rn), 1, 'yst', reads=['x1f'], writes=['y%d' % ti])
            outs_keys.append('y%d' % ti)


def prep_shared(inp):
    f = np.float32
    d = {}
    d['w_ada'] = np.ascontiguousarray(inp['w_ada'][0], dtype=f)
    ba = np.asarray(inp['b_ada'][0], dtype=f)
    d['b_adaT'] = np.ascontiguousarray(ba.reshape(96, 128).T)
    d['b_ada_row'] = np.ascontiguousarray(ba.reshape(1, -1))
    d['w_in'] = np.ascontiguousarray(inp['w_in'][0], dtype=f)
    d['w_out'] = np.ascontiguousarray(inp['w_out'][0], dtype=f)
    d['w_pool'] = np.ascontiguousarray(inp['w_pool'][0], dtype=f)
    d['pool_scaleT'] = np.ascontiguousarray(np.asarray(inp['pool_scale'][0], dtype=f).reshape(8, 128).T)
    lr = np.asarray(inp['lambda_re'][0], dtype=f).T
    li = np.asarray(inp['lambda_im'][0], dtype=f).T
    lam = np.stack([np.concatenate([lr, lr], 0), np.concatenate([li, li], 0)], 1)
    d['lamst'] = np.ascontiguousarray(lam)
    d['logdt_row'] = np.ascontiguousarray(np.asarray(inp['log_dt'][0], dtype=f).reshape(1, 64))
    br = np.asarray(inp['ssm_b_re'][0], dtype=f).transpose(1, 0, 2)
    bi = np.asarray(inp['ssm_b_im'][0], dtype=f).transpose(1, 0, 2)
    d['Bst'] = np.ascontiguousarray(np.stack([np.concatenate([br, bi], 0), np.concatenate([bi, br], 0)], 0).reshape(2, 128, 1024))
    cr = np.asarray(inp['ssm_c_re'][0], dtype=f).transpose(2, 0, 1)
    ci = np.asarray(inp['ssm_c_im'][0], dtype=f).transpose(2, 0, 1)
    d['Cst'] = np.ascontiguousarray(np.stack([np.concatenate([cr, ci], 0), np.concatenate([ci, cr], 0)], 0).reshape(2, 128, 1024))
    d['dskipT'] = np.ascontiguousarray(np.asarray(inp['d_skip'][0], dtype=f).reshape(8, 128).T)
    d['bgluT'] = np.ascontiguousarray(np.asarray(inp['b_glu'][0], dtype=f).reshape(8, 128).T)
    wg = np.asarray(inp['w_glu'][0], dtype=f)
    bd = np.zeros((8, 128, 128), f)
    for g in range(64):
        q = g % 8
        bd[g // 8, q * 16:(q + 1) * 16, q * 16:(q + 1) * 16] = wg[g]
    d['wglu_bd'] = bd
    d['lnrows'] = np.ascontiguousarray(np.stack([inp['ln1_g'][0], inp['ln1_b'][0], inp['ln2_g'][0], inp['ln2_b'][0]], 0), dtype=f)
    d['w_router'] = np.ascontiguousarray(inp['w_router'][0], dtype=f)
    d['b_router'] = np.ascontiguousarray(np.asarray(inp['b_router'][0], dtype=f).reshape(1, NE))
    c = np.zeros((128, 153), f)
    c[:, 0:128] = np.eye(128, dtype=f)
    c[:, 128:144] = 1.0 / (np.arange(16, dtype=f) + 1.0)
    for p in range(128):
        c[p, 144 + p // 16] = 1.0
    c[:64, 152] = 1.0
    c[64:, 152] = -1.0
    d['consts'] = c
    return d


def prep_moe_shared(inp):
    f = np.float32
    d = {}
    d['w_gu'] = np.ascontiguousarray(inp['w_gate_up'][0], dtype=f)
    d['w_dn'] = np.ascontiguousarray(inp['w_down'][0], dtype=f)
    bgu = np.asarray(inp['b_gate_up'][0], dtype=f)
    d['b_guT'] = np.ascontiguousarray(bgu.reshape(NE, 32, 128).transpose(2, 0, 1))
    d['b_dn'] = np.ascontiguousarray(inp['b_down'][0], dtype=f)
    return d


def prep_core(inp, b):
    f = np.float32
    d = {}
    xtok = np.concatenate([np.asarray(inp['x_prompt'][b], dtype=f), np.asarray(inp['x_sample'][b], dtype=f)], 0)
    d['xtok'] = np.ascontiguousarray(xtok)
    d['xT'] = np.ascontiguousarray(xtok.T)
    c2 = np.stack([np.asarray(inp['c_prompt'][b], dtype=f), np.asarray(inp['c_sample'][b], dtype=f)], -1)
    d['cT'] = np.ascontiguousarray(c2.reshape(16, 128, 2).transpose(1, 0, 2))
    d['cache_poolT'] = np.ascontiguousarray(np.asarray(inp['cache_pool'][0, b], dtype=f).T)
    sr = np.asarray(inp['state_ssm_re'][0, b], dtype=f).T
    si = np.asarray(inp['state_ssm_im'][0, b], dtype=f).T
    d['h0st'] = np.ascontiguousarray(np.concatenate([sr, si], 0))
    return d


_NC_CACHE = {}


def kernel(**inputs):
    n = 8
    if 'nc' not in _NC_CACHE:
        _NC_CACHE['nc'] = build_nc(debug=False, with_moe=True)
    nc = _NC_CACHE['nc']
    shared = prep_shared(inputs)
    shared.update(prep_moe_shared(inputs))
    in_maps = []
    for b in range(n):
        m = dict(shared)
        m.update(prep_core(inputs, b))
        in_maps.append(m)
    res = run_bass_kernel_spmd(nc, in_maps, core_ids=list(range(n)))
    R = res.results
    y = np.stack([r['y'] for r in R], 0)
    y_prompt = np.ascontiguousarray(y[:, :NP_, :])
    y_sample = np.ascontiguousarray(y[:, NP_:, :])
    npool = np.stack([r['npool'] for r in R], 0)
    nssm = np.stack([r['nssm'] for r in R], 0)
    new_pool_p = np.ascontiguousarray(npool[None, :, 0])
    new_pool_s = np.ascontiguousarray(npool[None, :, 1])
    re_p = np.ascontiguousarray(nssm[None, :, 0, :, 0:64])
    im_p = np.ascontiguousarray(nssm[None, :, 0, :, 64:128])
    re_s = np.ascontiguousarray(nssm[None, :, 1, :, 0:64])
    im_s = np.ascontiguousarray(nssm[None, :, 1, :, 64:128])
    return (y_prompt, y_sample, new_pool_p, re_p, im_p, new_pool_s, re_s, im_s)
```

```python
import numpy as np
from contextlib import ExitStack
import concourse.bass as bass
import concourse.mybir as mybir
from concourse.bass_utils import run_bass_kernel_spmd

F32 = mybir.dt.float32
BF16 = mybir.dt.bfloat16
ALU = mybir.AluOpType
AF = mybir.ActivationFunctionType

D = 2048
NP_ = 2048
NS_ = 16
TOK = NP_ + NS_
NE = 32
DN_ALPHA = 2.0 ** 0.25
LN_EPS = 1e-5
MAGIC = 12582912.0
CHUNKS = [(0, 512), (512, 512), (1024, 512), (1536, 512), (2048, 16)]
TT = [(i * 128, 128) for i in range(16)] + [(2048, 16)]
POOL_W = (2, 4, 8, 16)
DEBUG = False
KNOB = {}


class Prog:
    def __init__(self, nc, self_sync=True):
        self.nc = nc
        self.ops = []
        self.last_w = {}
        self.readers = {}
        self.self_sync = self_sync
        self.last_eng = {}
        self.last_dma = {}
        self.bar_pending = {}

    def barrier(self):
        snap = set(self.last_eng.values()) | set(self.last_dma.values())
        for e in ('pe', 'act', 'dve', 'pool', 'sp'):
            self.bar_pending[e] = set(snap) | self.bar_pending.get(e, set())

    def add(self, eng, fn, reads=(), writes=(), dma=0, semkey=None, indep=False):
        i = len(self.ops)
        deps = set()
        if eng in self.bar_pending:
            deps |= self.bar_pending.pop(eng)
        if dma:
            self.last_dma[semkey] = i
        else:
            self.last_eng[eng] = i
        for k in reads:
            j = self.last_w.get(k)
            if j is not None:
                deps.add(j)
        for k in writes:
            j = self.last_w.get(k)
            if j is not None:
                deps.add(j)
            rd = self.readers.get(k)
            if rd:
                deps.update(rd.values())
        rkey = ('dma', semkey) if dma else ('eng', eng)
        for k in reads:
            self.readers.setdefault(k, {})[rkey] = i
        for k in writes:
            self.last_w[k] = i
            self.readers[k] = {}
        self.ops.append(dict(eng=eng, fn=fn, deps=deps, dma=dma, semkey=semkey, indep=indep))
        return i

    def pe(self, fn, reads=(), writes=()):
        return self.add('pe', fn, reads, writes)

    def act(self, fn, reads=(), writes=(), indep=False):
        return self.add('act', fn, reads, writes, indep=indep)

    def dve(self, fn, reads=(), writes=(), indep=False):
        return self.add('dve', fn, reads, writes, indep=indep)

    def pool(self, fn, reads=(), writes=(), indep=False):
        return self.add('pool', fn, reads, writes, indep=indep)

    def dma(self, q, fn, n, semkey, reads=(), writes=()):
        return self.add(q, fn, reads, writes, dma=n, semkey=semkey)

    def finalize(self, final_reads=()):
        nc = self.nc
        ops = self.ops
        self.add('sp', None, reads=final_reads)
        needed = [False] * len(ops)
        for i, o in enumerate(ops):
            for j in o['deps']:
                p = ops[j]
                if p['dma']:
                    continue
                if p['eng'] == o['eng'] and (o['eng'] == 'pe' or not self.self_sync):
                    continue
                needed[j] = True
        ms = {}
        ms_base = {}
        ms_cnt = {e: 0 for e in ('pe', 'act', 'dve', 'pool', 'sp')}
        dma_cnt = {}
        tok = {}

        class _Dummy:
            def __getattr__(self, name):
                return lambda *a, **k: _Dummy()

        for i, o in enumerate(ops):
            if o['dma']:
                c = dma_cnt.get(o['semkey'], 0) + 16 * o['dma']
                dma_cnt[o['semkey']] = c
                tok[i] = c
                continue
            nsub = 1
            if o['fn'] is not None and o['eng'] != 'pe' and self.self_sync and not o.get('indep'):
                r = o['fn'](_Dummy())
                nsub = len(r) if isinstance(r, (list, tuple)) else 1
            if nsub > 1:
                ms_base[i] = ms_cnt[o['eng']]
                ms_cnt[o['eng']] += nsub
                ms[i] = ms_cnt[o['eng']]
                o['nsub'] = nsub
            elif needed[i]:
                ms_cnt[o['eng']] += 1
                ms[i] = ms_cnt[o['eng']]
        with ExitStack() as st:
            esem = {e: st.enter_context(nc.semaphore('s_' + e)) for e in ms_cnt}
            dsem = {}
            for k in dma_cnt:
                dsem[k] = st.enter_context(nc.semaphore('d%d' % len(dsem)))
            block = st.enter_context(nc.Block())

            def emit(ename, eng):
                waited = {}
                for i, o in enumerate(ops):
                    if o['eng'] != ename:
                        continue
                    need = {}
                    for j in o['deps']:
                        p = ops[j]
                        if p['dma']:
                            s = dsem[p['semkey']]
                            v = tok[j]
                        else:
                            if p['eng'] == ename and (ename == 'pe' or not self.self_sync):
                                continue
                            s = esem[p['eng']]
                            v = ms[j]
                        if need.get(s, 0) < v:
                            need[s] = v
                    for s, v in need.items():
                        if waited.get(s, 0) < v:
                            eng.wait_ge(s, v)
                            waited[s] = v
                    if o['fn'] is None:
                        continue
                    if o.get('nsub'):
                        sem_e = esem[ename]
                        base = ms_base[i]
                        st_ = {'k': 0}

                        class _Proxy:
                            def __getattr__(self_, name):
                                real = getattr(eng, name)

                                def wrapped(*a, **kw):
                                    if st_['k'] > 0:
                                        eng.wait_ge(sem_e, base + st_['k'])
                                    ins = real(*a, **kw)
                                    ins.then_inc(sem_e, 1)
                                    st_['k'] += 1
                                    return ins
                                return wrapped
                        o['fn'](_Proxy())
                        assert st_['k'] == o['nsub'], (st_['k'], o['nsub'])
                        waited[sem_e] = max(waited.get(sem_e, 0), base + o['nsub'] - 1)
                        continue
                    r = o['fn'](eng)
                    if o['dma']:
                        assert isinstance(r, (list, tuple)) and len(r) == o['dma'], (len(r), o['dma'])
                        for ins in r:
                            ins.then_inc(dsem[o['semkey']], 16)
                    elif needed[i]:
                        if isinstance(r, (list, tuple)):
                            r = r[-1]
                        r.then_inc(esem[ename], 1)

            @block.tensor
            def _(e):
                emit('pe', e)

            @block.scalar
            def _(e):
                emit('act', e)

            @block.vector
            def _(e):
                emit('dve', e)

            @block.gpsimd
            def _(e):
                emit('pool', e)

            @block.sync
            def _(e):
                emit('sp', e)


def build_nc(debug=False, with_moe=True, stop_after=None):
    nc = bass.Bass("TRN2", target_bir_lowering=False)

    def din(name, shape, dt=F32):
        return nc.dram_tensor(name, list(shape), dt, kind="ExternalInput").ap()

    def dout(name, shape, dt=F32):
        return nc.dram_tensor(name, list(shape), dt, kind="ExternalOutput").ap()

    def dscr(name, shape, dt=F32):
        return nc.dram_tensor(name, list(shape), dt, kind="Internal").ap()

    xT = din("xT", [D, TOK])
    xtok = din("xtok", [TOK, D])
    cT = din("cT", [128, 16, 2])
    w_ada = din("w_ada", [D, 6 * D])
    b_adaT = din("b_adaT", [128, 96])
    b_ada_row = din("b_ada_row", [1, 6 * D])
    w_in = din("w_in", [D, D])
    w_out = din("w_out", [D, D])
    w_pool = din("w_pool", [4, 256, 256])
    pool_scaleT = din("pool_scaleT", [128, 8])
    cache_poolT = din("cache_poolT", [1024, 15])
    lamst = din("lamst", [128, 2, 64])
    logdt_row = din("logdt_row", [1, 64])
    Bst = din("Bst", [2, 128, 1024])
    Cst = din("Cst", [2, 128, 1024])
    h0st = din("h0st", [128, 64])
    dskipT = din("dskipT", [128, 8])
    bgluT = din("bgluT", [128, 8])
    wglu_bd = din("wglu_bd", [8, 128, 128])
    lnrows = din("lnrows", [4, D])
    w_router = din("w_router", [D, NE])
    b_router = din("b_router", [1, NE])
    if with_moe:
        w_gu = din("w_gu", [KNOB.get('nexp', NE), D, 2 * D])
        w_dn = din("w_dn", [KNOB.get('nexp', NE), D, D])
        b_guT = din("b_guT", [128, NE, 32])
        b_dn = din("b_dn", [NE, D])
    consts = din("consts", [128, 128 + 16 + 8 + 1])

    y_out = dout("y", [TOK, D])
    npool = dout("npool", [2, 15, 1024])
    nssm = dout("nssm", [2, 64, 128])
    dbg = {}
    if debug:
        dbg['uT'] = dout("dbg_uT", [D, TOK])
        dbg['mixinT'] = dout("dbg_mixinT", [D, TOK])
        dbg['x1'] = dout("dbg_x1", [TOK, D])
        dbg['gates'] = dout("dbg_gates", [TOK, NE])
        dbg['modpp'] = dout("dbg_modpp", [128, 128])

    modrow = dscr("modrow", [4, D])
    x1_scr = dscr("x1_scr", [TOK, D])
    h2T_scr = dscr("h2T_scr", [128, 16, TOK], F32)

    P = Prog(nc)
    outs_keys = []

    with ExitStack() as st:
        def sb(name, shape, dt):
            return st.enter_context(nc.sbuf_tensor(name, list(shape), dt))

        AW = 44000
        arena = sb("arena", [128, AW], F32)

        def carve(off, words, dt=F32):
            assert off + words <= AW, (off, words)
            v = arena[:, off:off + words]
            if dt == BF16:
                v = v.bitcast(BF16)
            return v

        pb = [st.enter_context(nc.psum_tensor("pb%d" % i, [128, 512], F32)) for i in range(4)]
        pd = [st.enter_context(nc.psum_tensor("pd%d" % i, [128, 1024], F32)) for i in range(2)]
        pb += [pd[0][:, 0:512], pd[0][:, 512:1024], pd[1][:, 0:512], pd[1][:, 512:1024]]
        cst = sb("cst", [128, 153], F32)
        ident = cst[:, 0:128]
        invc = cst[:, 128:144]
        rowmask = cst[:, 144:152]
        sgn = cst[:, 152:153]
        modpp = sb("modpp", [128, 4, 16, 2], F32)
        gates = sb("gates", [128, 17, NE], F32)
        usave = sb("usave", [128, 8, 32], F32)
        small = sb("small", [128, 64], F32)
        P.dma('sp', lambda e: [e.dma_start(out=cst[:], in_=consts[:, :])], 1, 'cst', writes=['cst'])

        cT_s = sb("cT_s", [128, 16, 2], F32)
        sil = sb("sil", [128, 16, 2], F32)
        sil2 = sb("sil2", [128, 16, 2], BF16)
        badaT = sb("badaT", [128, 96], F32)
        srep = [carve(8192 + w * 1024, 1024, BF16).rearrange("p (k n) -> p k n", k=16) for w in range(2)]
        wada = [carve(b * 4096, 4096, BF16).rearrange("p (k n) -> p k n", k=16) for b in range(2)]
        bbc = [carve(10240 + b * 512, 512) for b in range(2)]
        gstg = [carve(11264 + b * 512, 512) for b in range(2)]
        P.dma('sp', lambda e: [e.dma_start(out=cT_s[:], in_=cT[:, :, :]), e.dma_start(out=badaT[:], in_=b_adaT[:, :])],
              2, 'cT', writes=['cT', 'badaT'])
        P.act(lambda e: e.activation(sil[:], cT_s[:], AF.Sigmoid), reads=['cT'], writes=['sil'])
        P.dve(lambda e: e.tensor_tensor(sil[:], sil[:], cT_s[:], op=ALU.mult), reads=['sil', 'cT'], writes=['sil'])
        P.dve(lambda e: e.tensor_copy(sil2[:], sil[:]), reads=['sil'], writes=['sil2'])
        for w in range(2):
            P.dve((lambda w: lambda e: e.tensor_copy(srep[w], sil[:, :, w:w + 1].to_broadcast([128, 16, 128])))(w),
                  reads=['sil'], writes=['srep%d' % w])
        pp_map = {0: 0, 1: 1, 3: 2, 4: 3}
        row_map = {2: 0, 5: 2}
        for j in range(24):
            sec, q = j // 4, j % 4
            b = j % 2
            P.dma('pool', (lambda j, b: lambda e: [e.dma_start(
                out=wada[b], in_=w_ada[:, j * 512:(j + 1) * 512].rearrange("(k p) c -> p k c", p=128))])(j, b),
                1, 'wada%d' % b, writes=['wada%d' % b])
            if sec in pp_map:
                s4 = pp_map[sec]
                for sub in range(4):
                    col = sec * 16 + q * 4 + sub
                    kdx = q * 4 + sub
                    pbk = 'pb%d' % (sub % 2)
                    P.pe((lambda b, sub: lambda e: [e.matmul(pb[sub % 2][:, 0:2], lhsT=wada[b][:, kd, sub * 128:(sub + 1) * 128],
                                                             rhs=sil2[:, kd, :], start=(kd == 0), stop=(kd == 15))
                                                    for kd in range(16)])(b, sub),
                         reads=['wada%d' % b, 'sil2'], writes=[pbk])
                    one = 1.0 if sec in (1, 4) else 0.0
                    P.dve((lambda s4, kdx, col, sub, one: lambda e: e.tensor_scalar(
                        modpp[:, s4, kdx, :], pb[sub % 2][:, 0:2], badaT[:, col:col + 1], one, op0=ALU.add, op1=ALU.add))(s4, kdx, col, sub, one),
                        reads=[pbk, 'badaT'], writes=['modpp'])
            else:
                P.dma('sp', (lambda j, b: lambda e: [e.dma_start(
                    out=bbc[b], in_=b_ada_row[0:1, j * 512:(j + 1) * 512].to_broadcast([128, 512]))])(j, b),
                    1, 'bbc%d' % b, writes=['bbc%d' % b])
                for w in range(2):
                    pbk = 'pb%d' % (2 + w)
                    P.pe((lambda b, w: lambda e: [e.matmul(pb[2 + w][:, :], lhsT=srep[w][:, kd, :], rhs=wada[b][:, kd, :],
                                                           start=(kd == 0), stop=(kd == 15)) for kd in range(16)])(b, w),
                         reads=['wada%d' % b, 'srep%d' % w], writes=[pbk])
                    P.dve((lambda b, w: lambda e: e.tensor_tensor(gstg[w], pb[2 + w][:, :], bbc[b], op=ALU.add))(b, w),
                          reads=[pbk, 'bbc%d' % b], writes=['gstg%d' % w])
                    ridx = row_map[sec] + w
                    P.dma('sp', (lambda ridx, q, w: lambda e: [e.dma_start(
                        out=modrow[ridx:ridx + 1, q * 512:(q + 1) * 512], in_=gstg[w][0:1, :])])(ridx, q, w),
                        1, 'gstg%d' % w, reads=['gstg%d' % w], writes=['modrow'])
        if debug:
            P.dma('sp', lambda e: [e.dma_start(out=dbg['modpp'][:, :], in_=modpp[:].rearrange("p a k w -> p (a k w)"))],
                  1, 'dbgmod', reads=['modpp'], writes=['dbg_modpp'])
            outs_keys.append('dbg_modpp')

        if stop_after == 'A':
            P.finalize(final_reads=outs_keys)
            return nc
        O_HT = 0
        O_R2 = 16512
        O_UP = 23712
        O_US = 31968
        hT = carve(O_HT, 16512, BF16).rearrange("p (k n) -> p k n", k=16)
        mixinT = hT
        winr = [carve(O_R2 + i * 1024, 1024, BF16).rearrange("p (k n) -> p k n", k=16) for i in range(3)]
        xst = [carve(O_R2 + 3072 + i * 2064, 2064) for i in range(2)]
        uP = carve(O_UP, 8256, BF16).rearrange("p (k n) -> p k n", k=8)
        uS = carve(O_US, 8256, BF16).rearrange("p (k n) -> p k n", k=8)
        P.barrier()
        for kd in range(16):
            b = kd % 2
            P.dma('sp', (lambda kd, b: lambda e: [e.dma_start(out=xst[b], in_=xT[kd * 128:(kd + 1) * 128, :])])(kd, b),
                  1, 'xst%d' % b, writes=['xst%d' % b])
            extra = []
            P.act((lambda kd, b: lambda e: [
                e.activation(hT[:, kd, 0:NP_], xst[b][:, 0:NP_], AF.Identity, bias=modpp[:, 0, kd, 0:1], scale=modpp[:, 1, kd, 0:1]),
                e.activation(hT[:, kd, NP_:TOK], xst[b][:, NP_:TOK], AF.Identity, bias=modpp[:, 0, kd, 1:2], scale=modpp[:, 1, kd, 1:2]),
            ])(kd, b), reads=['xst%d' % b, 'modpp'], writes=['hT'] + extra)
        if stop_after == 'B1':
            P.finalize(final_reads=outs_keys)
            return nc
        ev = 0
        for m in range(KNOB.get('nm', 16)):
            r = m % 3
            P.dma('pool', (lambda m, r: lambda e: [e.dma_start(
                out=winr[r], in_=w_in[:, m * 128:(m + 1) * 128].rearrange("(k p) c -> p k c", p=128))])(m, r),
                1, 'win%d' % r, writes=['win%d' % r])
            for ci, (c0, cn) in enumerate(CHUNKS):
                bk = ev % 2
                pbk = 'pb%d' % bk
                P.pe((lambda r, bk, c0, cn: lambda e: [e.matmul(pb[bk][:, 0:cn], lhsT=winr[r][:, kd, :], rhs=hT[:, kd, c0:c0 + cn],
                                                               start=(kd == 0), stop=(kd == 15)) for kd in range(16)])(r, bk, c0, cn),
                     reads=['win%d' % r, 'hT'], writes=[pbk])
                dst = uP[:, m, c0:c0 + cn] if m < 8 else uS[:, m - 8, c0:c0 + cn]
                dkey = 'uT%d' % m
                if ev % 2 == 0:
                    P.act((lambda dst, bk, cn: lambda e: e.copy(dst, pb[bk][:, 0:cn]))(dst, bk, cn), reads=[pbk], writes=[dkey])
                else:
                    P.dve((lambda dst, bk, cn: lambda e: e.tensor_copy(dst, pb[bk][:, 0:cn]))(dst, bk, cn), reads=[pbk], writes=[dkey])
                if m < 8 and ci == 3 and not KNOB.get('nousave'):
                    P.dve((lambda m, bk: lambda e: e.tensor_copy(usave[:, m, 0:16], pb[bk][:, 496:512]))(m, bk), reads=[pbk, dkey], writes=['usave'])
                if m < 8 and ci == 4 and not KNOB.get('nousave'):
                    P.dve((lambda m, bk: lambda e: e.tensor_copy(usave[:, m, 16:32], pb[bk][:, 0:16]))(m, bk), reads=[pbk, dkey], writes=['usave'])
                ev += 1
        if stop_after == 'B2':
            P.finalize(final_reads=outs_keys)
            return nc
        nps = carve(40224, 1024)
        for m in range(8):
            P.pe((lambda m: lambda e: e.transpose(pb[2 + (m % 2)][0:32, 0:128], usave[:, m, :], ident))(m),
                 reads=['usave', 'cst'], writes=['pb%d' % (2 + (m % 2))])
            P.dve((lambda m: lambda e: e.tensor_copy(nps[0:32, m * 128:(m + 1) * 128], pb[2 + (m % 2)][0:32, 0:128]))(m),
                  reads=['pb%d' % (2 + (m % 2))], writes=['nps'])
        P.dma('sp', lambda e: [e.dma_start(out=npool[0], in_=nps[1:16, :]), e.dma_start(out=npool[1], in_=nps[17:32, :])],
              2, 'npool', reads=['nps'], writes=['npool'])
        outs_keys.append('npool')
        if debug:
            for m in range(16):
                src = uP[:, m, :] if m < 8 else uS[:, m - 8, :]
                P.dve((lambda src: lambda e: e.tensor_copy(xst[0], src))(src), reads=['uT%d' % m], writes=['xst0'])
                P.dma('sp', (lambda m: lambda e: [e.dma_start(out=dbg['uT'][m * 128:(m + 1) * 128, :], in_=xst[0])])(m),
                      1, 'xst0', reads=['xst0'], writes=['dbg_uT%d' % m])
                outs_keys.append('dbg_uT%d' % m)

        if stop_after == 'B':
            P.finalize(final_reads=outs_keys)
            return nc
        E0 = carve(O_R2, 2048)
        E1 = carve(O_R2 + 2048, 2048)
        diffT = carve(O_R2 + 4096, 2064, BF16).rearrange("p (k n) -> p k n", k=2)
        wpl = [carve(O_R2 + 6160 + i * 256, 256, BF16).rearrange("p (k n) -> p k n", k=2) for i in range(2)]
        Es = [sb("Es%d" % i, [128, 31], F32) for i in range(2)]
        pscT = sb("pscT", [128, 8], F32)
        P.dma('sp', lambda e: [e.dma_start(out=pscT[:], in_=pool_scaleT[:, :])], 1, 'pscT', writes=['pscT'])
        B_KEYS = []
        P.barrier()
        first = True
        for g in range(4):
            wdw = POOL_W[g]
            nst = {2: 1, 4: 2, 8: 3, 16: 4}[wdw]
            P.dma('pool', (lambda g: lambda e: [e.dma_start(out=wpl[g % 2], in_=w_pool[g].rearrange("(k p) c -> p k c", p=128))])(g),
                  1, 'wpl%d' % (g % 2), writes=['wpl%d' % (g % 2)] + (B_KEYS if first else []))
            for i in range(2):
                m = 2 * g + i
                uk = 'uT%d' % m
                src = uP[:, m, 0:NP_]
                bufs = [E0, E1]
                cur = None
                k = 1
                for s_ in range(nst):
                    dstb = bufs[s_ % 2]
                    a_in = src if cur is None else cur
                    rk = [uk] if cur is None else ['E%d' % ((s_ + 1) % 2)]
                    P.dve((lambda dstb, a_in, k: lambda e: [
                        e.tensor_tensor(dstb[:, k:NP_], a_in[:, k:NP_], a_in[:, 0:NP_ - k], op=ALU.add),
                        e.tensor_copy(dstb[:, 0:k], a_in[:, 0:k])])(dstb, a_in, k),
                        reads=rk, writes=['E%d' % (s_ % 2)] + (B_KEYS if first else []))
                    first = False
                    cur = dstb
                    k *= 2
                ck = 'E%d' % ((nst - 1) % 2)
                other = bufs[nst % 2]
                ok = 'E%d' % (nst % 2)
                P.dve((lambda cur, src, i, wdw: lambda e: e.scalar_tensor_tensor(
                    diffT[:, i, 0:NP_], in0=cur[:, 0:NP_], scalar=1.0 / wdw, in1=src, op0=ALU.mult, op1=ALU.subtract))(cur, src, i, wdw),
                    reads=[ck, uk], writes=['diffT%d' % i])
                P.dve((lambda cur, other, src, i, wdw: lambda e: [
                    e.tensor_tensor(other[:, 0:wdw - 1], cur[:, 0:wdw - 1], invc[:, 0:wdw - 1], op=ALU.mult),
                    e.tensor_tensor(diffT[:, i, 0:wdw - 1], other[:, 0:wdw - 1], src[:, 0:wdw - 1], op=ALU.subtract)])(cur, other, src, i, wdw),
                    reads=[ck, uk, 'cst', ok], writes=['diffT%d' % i, ok])
                P.dma('sp', (lambda m: lambda e: [e.dma_start(out=Es[0][:, 0:15], in_=cache_poolT[m * 128:(m + 1) * 128, :])])(m),
                      1, 'Es0', writes=['Es0'])
                P.dve((lambda m: lambda e: e.tensor_copy(Es[0][:, 15:31], usave[:, m, 16:32]))(m), reads=['usave'], writes=['Es0'])
                k = 1
                cs = 0
                for s_ in range(nst):
                    P.dve((lambda cs, k: lambda e: e.tensor_tensor(Es[1 - cs][:, 2 * k - 1:31], Es[cs][:, 2 * k - 1:31], Es[cs][:, k - 1:31 - k], op=ALU.add))(cs, k),
                          reads=['Es%d' % cs], writes=['Es%d' % (1 - cs)])
                    cs = 1 - cs
                    k *= 2
                P.dve((lambda cs, m, i, wdw: lambda e: e.scalar_tensor_tensor(
                    diffT[:, i, NP_:TOK], in0=Es[cs][:, 15:31], scalar=1.0 / wdw, in1=usave[:, m, 16:32], op0=ALU.mult, op1=ALU.subtract))(cs, m, i, wdw),
                    reads=['Es%d' % cs, 'usave'], writes=['diffT%d' % i])
            for j in range(2):
                mo = 2 * g + j
                for ci, (c0, cn) in enumerate(CHUNKS):
                    bk = ev % 2
                    pbk = 'pb%d' % bk
                    P.pe((lambda g, j, bk, c0, cn: lambda e: [e.matmul(pb[bk][:, 0:cn], lhsT=wpl[g % 2][:, i, j * 128:(j + 1) * 128],
                                                                      rhs=diffT[:, i, c0:c0 + cn], start=(i == 0), stop=(i == 1)) for i in range(2)])(g, j, bk, c0, cn),
                         reads=['wpl%d' % (g % 2), 'diffT0', 'diffT1'], writes=[pbk])
                    P.act((lambda mo, bk, c0, cn: lambda e: e.activation(mixinT[:, mo, c0:c0 + cn], pb[bk][:, 0:cn], AF.Copy, scale=pscT[:, mo:mo + 1]))(mo, bk, c0, cn),
                          reads=[pbk, 'pscT', 'hT'], writes=['mix%d' % mo])
                    ev += 1

        if stop_after == 'C':
            P.finalize(final_reads=outs_keys)
            return nc
        P.barrier()
        lam = sb("lam", [128, 2, 64], F32)
        ldt = sb("ldt", [128, 64], F32)
        prm = carve(16512 + 12448 + 2192, 768).rearrange("p (a n) -> p a n", a=12)
        MAGt = sb("MAGt", [128, 64], F32)
        THt = sb("THt", [128, 64], F32)
        K1 = sb("K1", [128, 64], F32)
        K2 = sb("K2", [128, 64], F32)
        h0s = sb("h0s", [128, 64], F32)
        dsk = sb("dsk", [128, 8], F32)
        bgl = sb("bgl", [128, 8], F32)
        G1l = sb("G1l", [128, 2, 64], F32)
        G2l = sb("G2l", [128, 2, 64], F32)
        perm = sb("perm", [128, 128], F32)
        halfpi = sb("halfpi", [128, 1], F32)
        P.dma('sp', lambda e: [e.dma_start(out=lam[:], in_=lamst[:, :, :]),
                               e.dma_start(out=ldt[:], in_=logdt_row[0:1, :].to_broadcast([128, 64])),
                               e.dma_start(out=h0s[:], in_=h0st[:, :]),
                               e.dma_start(out=dsk[:], in_=dskipT[:, :]),
                               e.dma_start(out=bgl[:], in_=bgluT[:, :])], 5, 'ssmp', writes=['ssmp'])
        lr, li = lam[:, 0, :], lam[:, 1, :]
        dt_, lrdt, th, fr, cs_, sn_, are, aim, den, t1, t2, t3 = [prm[:, i, :] for i in range(12)]

        def pm():
            def f(e):
                r = []
                r.append(e.memset(halfpi[:], float(np.pi / 2)))
                return r
            return f
        P.pool(lambda e: e.memset(halfpi[:], float(np.pi / 2)), writes=['halfpi'])
        P.act(lambda e: e.activation(dt_, ldt[:], AF.Exp), reads=['ssmp'], writes=['p_dt'])
        P.dve(lambda e: [e.tensor_tensor(lrdt, lr, dt_, op=ALU.mult),
                         e.tensor_tensor(th, li, dt_, op=ALU.mult)], reads=['p_dt', 'ssmp'], writes=['p_a'])
        P.act(lambda e: e.activation(MAGt[:], lrdt, AF.Exp), reads=['p_a'], writes=['MAG'])
        P.dve(lambda e: [e.tensor_scalar(th, th, float(1.0 / (2 * np.pi)), None, op0=ALU.mult),
                         e.tensor_scalar(fr, th, MAGIC, None, op0=ALU.add),
                         e.tensor_scalar(fr, fr, -MAGIC, None, op0=ALU.add),
                         e.tensor_tensor(THt[:], th, fr, op=ALU.subtract)], reads=['p_a'], writes=['TH'])
        P.act(lambda e: [e.activation(sn_, THt[:], AF.Sin, scale=float(2 * np.pi)),
                         e.activation(t1, THt[:], AF.Abs),
                         e.activation(cs_, t1, AF.Sin, bias=halfpi[:, 0:1], scale=float(-2 * np.pi))],
              reads=['TH', 'halfpi'], writes=['p_trig'])
        P.dve(lambda e: [
            e.tensor_tensor(are, MAGt[:], cs_, op=ALU.mult),
            e.tensor_scalar(are, are, -1.0, None, op0=ALU.add),
            e.tensor_tensor(aim, MAGt[:], sn_, op=ALU.mult),
            e.tensor_tensor(den, lr, lr, op=ALU.mult),
            e.tensor_tensor(t1, li, li, op=ALU.mult),
            e.tensor_tensor(den, den, t1, op=ALU.add),
            e.reciprocal(den, den),
            e.tensor_tensor(t1, are, lr, op=ALU.mult),
            e.tensor_tensor(t2, aim, li, op=ALU.mult),
            e.tensor_tensor(t1, t1, t2, op=ALU.add),
            e.tensor_tensor(K1[:], t1, den, op=ALU.mult),
            e.tensor_tensor(t1, aim, lr, op=ALU.mult),
            e.tensor_tensor(t2, are, li, op=ALU.mult),
            e.tensor_tensor(t1, t1, t2, op=ALU.subtract),
            e.tensor_tensor(t3, t1, den, op=ALU.mult),
            e.tensor_scalar(K2[:], t3, sgn[:, 0:1], -1.0, op0=ALU.mult, op1=ALU.mult),
            e.tensor_scalar(perm[:, 0:64], ident[:, 64:128], -1.0, None, op0=ALU.mult),
            e.tensor_copy(perm[:, 64:128], ident[:, 0:64]),
        ], reads=['MAG', 'p_trig', 'ssmp', 'cst'], writes=['Kp', 'perm'])

        O_D = O_R2
        CP = carve(O_D, 2080)
        SP = carve(O_D + 2080, 2080)
        bpr = carve(O_D + 4160, 2064)
        iot = carve(O_D + 6224, 2080)
        yvF = carve(O_D + 8304, 2080)
        yv = yvF[:, 0:TOK]
        yg = carve(O_D + 10384, 2064)
        ang = yvF
        tmpr = yg
        gsc = yg
        O_G = O_D + 12448
        G1b = carve(O_G, 1032, BF16)
        G2b = carve(O_G + 1032, 1032, BF16)
        packs = carve(O_G + 2064, 128)
        O_T = 40224
        BB = carve(O_T, 1024)
        Clb = carve(O_T + 1024, 1024, BF16).rearrange("p (a n) -> p a n", a=2)
        LB = carve(O_T + 2048, 1024, BF16).rearrange("p (g a n) -> p g a n", g=8, a=2)
        LC = sb("LC", [128, 8, 2, 128], BF16)
        c5buf = carve(O_T + 3072, 512)
        ygb = G1b
        Bl = yv[:, 0:2048].rearrange("p (a n) -> p a n", a=2)
        Cl = yg[:, 0:2048].rearrange("p (a n) -> p a n", a=2)
        wgl = sb("wgl", [128, 128], BF16)
        C_KEYS = []
        P.pool(lambda e: e.iota(iot, pattern=[[1, 2080]], base=0, channel_multiplier=0, allow_small_or_imprecise_dtypes=True), writes=['iot'])
        P.dma('sp', lambda e: [e.dma_start(out=Bl[:, 0, :], in_=Bst[0]), e.dma_start(out=Bl[:, 1, :], in_=Bst[1]),
                               e.dma_start(out=Cl[:, 0, :], in_=Cst[0]), e.dma_start(out=Cl[:, 1, :], in_=Cst[1])], 4, 'BC', writes=['BC', 'yv', 'yg'])
        BB3 = BB.rearrange("p (g c) -> p g c", g=64)
        P.dve(lambda e: [
            e.tensor_tensor(BB3, Bl[:, 0, :].rearrange("p (g c) -> p g c", g=64), K1[:].unsqueeze(2).to_broadcast([128, 64, 16]), op=ALU.mult),
            e.tensor_tensor(Bl[:, 1, :].rearrange("p (g c) -> p g c", g=64), Bl[:, 1, :].rearrange("p (g c) -> p g c", g=64),
                            K2[:].unsqueeze(2).to_broadcast([128, 64, 16]), op=ALU.mult),
            e.tensor_tensor(BB, BB, Bl[:, 1, :], op=ALU.add),
            e.tensor_scalar(Clb[:, 0, :], Cl[:, 0, :], sgn[:, 0:1], None, op0=ALU.mult),
            e.tensor_scalar(Clb[:, 1, :], Cl[:, 1, :], -1.0, None, op0=ALU.mult),
        ], reads=['BC', 'Kp', 'cst', 'yv', 'yg'], writes=['BBC', 'yv', 'yg'])
        P.pool(lambda e: e.memset(LC[:], 0.0), writes=['LC'])
        for blk in range(8):
            ukey = 'uT%d' % (8 + blk)
            ut = uS[:, blk, :]
            P.pe((lambda blk: lambda e: e.transpose(pb[7][:, 0:128], BB[:, blk * 128:(blk + 1) * 128], ident))(blk),
                 reads=['BBC', 'cst'], writes=['pb7'])
            P.act(lambda e: e.copy(packs, pb[7][:, 0:128]), reads=['pb7'], writes=['packs'])

            def lbprep(blk):
                def f(e):
                    r = []
                    for gq in range(8):
                        r.append(e.tensor_scalar(LB[:, gq, 0, :], packs, rowmask[:, gq:gq + 1], None, op0=ALU.mult))
                        r.append(e.tensor_scalar(LB[:, gq, 1, 0:64], packs[:, 64:128], rowmask[:, gq:gq + 1], None, op0=ALU.mult))
                        r.append(e.tensor_scalar(LB[:, gq, 1, 64:128], packs[:, 0:64], rowmask[:, gq:gq + 1], -1.0, op0=ALU.mult, op1=ALU.mult))
                        g = blk * 8 + gq
                        r.append(e.tensor_copy(LC[:, gq, 0, gq * 16:(gq + 1) * 16], Clb[:, 0, g * 16:(g + 1) * 16]))
                        r.append(e.tensor_copy(LC[:, gq, 1, gq * 16:(gq + 1) * 16], Clb[:, 1, g * 16:(g + 1) * 16]))
                    return r
                return f
            P.dve(lbprep(blk), reads=['packs', 'BBC', 'cst'], writes=['LB', 'LC'], indep=True)
            for gq in range(8):
                g = blk * 8 + gq
                def ang_pool(g):
                    P.pool((lambda g: lambda e: [
                        e.tensor_scalar(ang, iot, THt[:, g:g + 1], MAGIC, op0=ALU.mult, op1=ALU.add),
                        e.tensor_scalar(ang, ang, -MAGIC, None, op0=ALU.add)])(g),
                        reads=['iot', 'TH'], writes=['yv'])

                def ang_dve(g):
                    P.dve((lambda g: lambda e: e.scalar_tensor_tensor(ang, in0=iot, scalar=THt[:, g:g + 1], in1=ang, op0=ALU.mult, op1=ALU.subtract))(g),
                          reads=['iot', 'TH', 'yv'], writes=['yv'])
                if gq == 0:
                    ang_pool(g)
                    ang_dve(g)
                P.act(lambda e: [e.activation(SP, ang, AF.Sin, scale=float(2 * np.pi)),
                                 e.activation(ang, ang, AF.Abs),
                                 e.activation(CP, ang, AF.Sin, bias=halfpi[:, 0:1], scale=float(-2 * np.pi))],
                      reads=['yv', 'halfpi'], writes=['CP', 'SP', 'yv'])
                if gq < 7:
                    ang_pool(g + 1)
                for ci, (c0, cn) in enumerate(CHUNKS):
                    t0 = c0 if ci < 4 else 1
                    P.pe((lambda ut, gq, c0, cn: lambda e: [
                        e.matmul(pb[5][:, 0:cn], lhsT=LB[:, gq, 0, :], rhs=ut[:, c0:c0 + cn], start=True, stop=True),
                        e.matmul(pb[6][:, 0:cn], lhsT=LB[:, gq, 1, :], rhs=ut[:, c0:c0 + cn], start=True, stop=True)])(ut, gq, c0, cn),
                        reads=['LB', ukey], writes=['pb5', 'pb6'])
                    P.act((lambda cn: lambda e: e.copy(c5buf[:, 0:cn], pb[5][:, 0:cn]))(cn), reads=['pb5'], writes=['c5buf'])
                    P.pool((lambda c0, cn, t0: lambda e: e.tensor_tensor(tmpr[:, c0:c0 + cn], c5buf[:, 0:cn], CP[:, t0:t0 + cn], op=ALU.mult))(c0, cn, t0),
                           reads=['c5buf', 'CP'], writes=['yg'])
                    P.dve((lambda c0, cn, t0: lambda e: e.tensor_tensor(bpr[:, c0:c0 + cn], pb[6][:, 0:cn], SP[:, t0:t0 + cn], op=ALU.mult))(c0, cn, t0),
                          reads=['pb6', 'SP'], writes=['bpr'])
                    P.dve((lambda c0, cn: lambda e: e.tensor_tensor(bpr[:, c0:c0 + cn], bpr[:, c0:c0 + cn], tmpr[:, c0:c0 + cn], op=ALU.add))(c0, cn),
                          reads=['bpr', 'yg'], writes=['bpr'])
                if gq < 7:
                    ang_dve(g + 1)
                P.dve((lambda g: lambda e: [
                    e.tensor_tensor_scan(gsc[:, 0:NP_], MAGt[:, g:g + 1].to_broadcast([128, NP_]), bpr[:, 0:NP_], 0.0, op0=ALU.mult, op1=ALU.add),
                    e.tensor_tensor_scan(gsc[:, NP_:TOK], MAGt[:, g:g + 1].to_broadcast([128, NS_]), bpr[:, NP_:TOK], h0s[:, g:g + 1], op0=ALU.mult, op1=ALU.add)])(g),
                    reads=['bpr', 'MAG', 'ssmp'], writes=['yg'], indep=True)
                P.dve((lambda g: lambda e: [
                    e.tensor_tensor(G1b[:, 0:NP_], gsc[:, 0:NP_], CP[:, 0:NP_], op=ALU.mult),
                    e.tensor_tensor(G1b[:, NP_:TOK], gsc[:, NP_:TOK], CP[:, 1:1 + NS_], op=ALU.mult),
                    e.tensor_tensor(G1l[:, 0, g:g + 1], gsc[:, NP_ - 1:NP_], CP[:, NP_ - 1:NP_], op=ALU.mult),
                    e.tensor_tensor(G1l[:, 1, g:g + 1], gsc[:, TOK - 1:TOK], CP[:, NS_:NS_ + 1], op=ALU.mult),
                    e.tensor_tensor(G2l[:, 0, g:g + 1], gsc[:, NP_ - 1:NP_], SP[:, NP_ - 1:NP_], op=ALU.mult),
                    e.tensor_tensor(G2l[:, 1, g:g + 1], gsc[:, TOK - 1:TOK], SP[:, NS_:NS_ + 1], op=ALU.mult)])(g),
                    reads=['yg', 'CP', 'SP'], writes=['G1b', 'Gl'], indep=True)
                P.pool(lambda e: [
                    e.tensor_tensor(G2b[:, 0:NP_], gsc[:, 0:NP_], SP[:, 0:NP_], op=ALU.mult),
                    e.tensor_tensor(G2b[:, NP_:TOK], gsc[:, NP_:TOK], SP[:, 1:1 + NS_], op=ALU.mult)],
                    reads=['yg', 'SP'], writes=['G2b'], indep=True)
                for ci, (c0, cn) in enumerate(CHUNKS):
                    P.pe((lambda gq, ci, c0, cn: lambda e: [
                        e.matmul(pb[ci][:, 0:cn], lhsT=LC[:, gq, 0, :], rhs=G1b[:, c0:c0 + cn], start=(gq == 0), stop=False),
                        e.matmul(pb[ci][:, 0:cn], lhsT=LC[:, gq, 1, :], rhs=G2b[:, c0:c0 + cn], start=False, stop=(gq == 7))])(gq, ci, c0, cn),
                        reads=['LC', 'G1b', 'G2b'], writes=['pb%d' % ci])
            P.dma('pool', (lambda blk: lambda e: [e.dma_start(out=wgl[:], in_=wglu_bd[blk])])(blk), 1, 'wgl', writes=['wgl'])
            for ci, (c0, cn) in enumerate(CHUNKS):
                P.dve((lambda ut, blk, ci, c0, cn: lambda e: e.scalar_tensor_tensor(
                    yv[:, c0:c0 + cn], in0=ut[:, c0:c0 + cn], scalar=dsk[:, blk:blk + 1], in1=pb[ci][:, 0:cn], op0=ALU.mult, op1=ALU.add))(ut, blk, ci, c0, cn),
                    reads=['pb%d' % ci, ukey, 'ssmp'], writes=['yv'])
            P.dve(lambda e: [
                e.tensor_tensor(yg, yv, yv, op=ALU.mult),
                e.tensor_scalar(yg, yg, 0.044715, 1.0, op0=ALU.mult, op1=ALU.add),
                e.tensor_tensor(yg, yg, yv, op=ALU.mult)], reads=['yv'], writes=['yg'])
            P.act(lambda e: e.activation(yg, yg, AF.Sigmoid, scale=1.5957691216057308), reads=['yg'], writes=['yg'])
            P.dve(lambda e: [e.tensor_tensor(yg, yg, yv, op=ALU.mult),
                             e.tensor_copy(ygb, yg)], reads=['yg', 'yv'], writes=['yg', 'G1b'])
            for ci, (c0, cn) in enumerate(CHUNKS):
                bk = 5 + (ci % 2)
                P.pe((lambda bk, c0, cn: lambda e: e.matmul(pb[bk][:, 0:cn], lhsT=wgl[:], rhs=ygb[:, c0:c0 + cn], start=True, stop=True))(bk, c0, cn),
                     reads=['wgl', 'G1b'], writes=['pb%d' % bk])
                P.act((lambda blk, bk, c0, cn: lambda e: e.activation(yv[:, c0:c0 + cn], pb[bk][:, 0:cn], AF.Sigmoid, bias=bgl[:, blk:blk + 1]))(blk, bk, c0, cn),
                      reads=['pb%d' % bk, 'ssmp'], writes=['yv'])
                P.dve((lambda blk, c0, cn: lambda e: e.tensor_tensor(mixinT[:, 8 + blk, c0:c0 + cn], yg[:, c0:c0 + cn], yv[:, c0:c0 + cn], op=ALU.mult))(blk, c0, cn),
                      reads=['yv', 'yg', 'hT'], writes=['mix%d' % (8 + blk)])
        nst_s = sb("nst_s", [64, 2, 128], F32)
        for w in range(2):
            P.pe((lambda w: lambda e: [e.matmul(pb[5 + w][0:64, 0:128], lhsT=G1l[:, w, :], rhs=ident, start=True, stop=False),
                                       e.matmul(pb[5 + w][0:64, 0:128], lhsT=G2l[:, w, :], rhs=perm[:], start=False, stop=True)])(w),
                 reads=['Gl', 'perm', 'cst'], writes=['pb%d' % (5 + w)])
            P.act((lambda w: lambda e: e.copy(nst_s[:, w, :], pb[5 + w][0:64, 0:128]))(w), reads=['pb%d' % (5 + w)], writes=['nst%d' % w])
            P.dma('sp', (lambda w: lambda e: [e.dma_start(out=nssm[w], in_=nst_s[:, w, :])])(w), 1, 'nst%d' % w, reads=['nst%d' % w], writes=['nssm%d' % w])
            outs_keys.append('nssm%d' % w)
        if debug:
            for m in range(16):
                P.dve((lambda m: lambda e: e.tensor_copy(yv, mixinT[:, m, :]))(m), reads=['mix%d' % m], writes=['yv'])
                P.dma('sp', (lambda m: lambda e: [e.dma_start(out=dbg['mixinT'][m * 128:(m + 1) * 128, :], in_=yv)])(m),
                      1, 'dbgyv', reads=['yv'], writes=['dbg_mix%d' % m])
                outs_keys.append('dbg_mix%d' % m)

        if stop_after == 'D':
            P.finalize(final_reads=outs_keys)
            return nc
        MIX_KEYS = ['mix%d' % m for m in range(16)]
        D_KEYS = ['CP', 'SP', 'bpr', 'tmpr', 'gsc', 'G1b', 'G2b', 'ang'] + ['uT%d' % m for m in range(8, 16)]
        P.barrier()
        O_E = O_R2
        wout = carve(O_E, 16384, BF16).rearrange("p (k n) -> p k n", k=16)
        O_E2 = O_E + 16384
        g1b = [carve(O_E2, 2048), carve(O_E2, 2048)]
        lng = carve(O_E2 + 2048, 2048)
        lnb = carve(O_E2 + 4096, 2048)
        x1t = carve(O_E2 + 6144, 2048)
        h2fl = carve(O_E2 + 8192, 2048)
        h2f = h2fl.rearrange("p (k n) -> p k n", k=16)
        wr_s = carve(O_E2 + 10240, 512).rearrange("p (k n) -> p k n", k=16)
        brt = sb("brt", [128, NE], F32)
        stats = sb("stats", [128, 4, 6], F32)
        mv = sb("mv", [128, 2], F32)
        top8 = sb("top8", [128, 8], F32)
        lg = sb("lg", [128, NE], F32)
        for q in range(4):
            P.dma('pool', (lambda q: lambda e: [e.dma_start(out=wout[:, :, q * 512:(q + 1) * 512],
                                                            in_=w_out[:, q * 512:(q + 1) * 512].rearrange("(k p) c -> p k c", p=128))])(q),
                  1, 'wout', writes=['wout'])
        P.dma('sp', lambda e: [e.dma_start(out=g1b[0], in_=modrow[0:1, :].to_broadcast([128, D])),
                               e.dma_start(out=lng, in_=lnrows[0:1, :].to_broadcast([128, D])),
                               e.dma_start(out=lnb, in_=lnrows[1:2, :].to_broadcast([128, D])),
                               e.dma_start(out=wr_s[:], in_=w_router.rearrange("(k p) n -> p k n", p=128)),
                               e.dma_start(out=brt[:], in_=b_router[0:1, :].to_broadcast([128, NE]))],
              5, 'Eld', reads=['modrow'], writes=['Eld', 'g1b'])
        for ti, (r0, rn) in enumerate(TT):
            w = 0 if ti < 16 else 1
            if ti == 16:
                P.dma('sp', lambda e: [e.dma_start(out=g1b[0], in_=modrow[1:2, :].to_broadcast([128, D]))], 1, 'g1b2', reads=['modrow'], writes=['g1b'])
            P.dma('sp', (lambda r0, rn: lambda e: [e.dma_start(out=x1t[0:rn, :], in_=xtok[r0:r0 + rn, :])])(r0, rn),
                  1, 'xt_s', writes=['x1t'])
            for q in range(4):
                P.pe((lambda q, r0, rn: lambda e: [e.matmul(pb[q][0:rn, :], lhsT=mixinT[:, m, r0:r0 + rn], rhs=wout[:, m, q * 512:(q + 1) * 512],
                                                            start=(m == 0), stop=(m == 15)) for m in range(16)])(q, r0, rn),
                     reads=MIX_KEYS + ['wout'], writes=['pb%d' % q])
                P.dve((lambda q, rn, w: lambda e: e.tensor_tensor(h2fl[0:rn, q * 512:(q + 1) * 512], pb[q][0:rn, :], g1b[w][0:rn, q * 512:(q + 1) * 512], op=ALU.mult))(q, rn, w),
                      reads=['pb%d' % q, 'Eld', 'g1b'], writes=['h2f0', 'h2f1', 'h2f2', 'h2f3'])
            P.dve((lambda rn: lambda e: [
                e.scalar_tensor_tensor(x1t[0:rn, :], in0=x1t[0:rn, :], scalar=float(DN_ALPHA), in1=h2fl[0:rn, :], op0=ALU.mult, op1=ALU.add),
            ] + [e.bn_stats(stats[0:rn, q, :], x1t[0:rn, q * 512:(q + 1) * 512]) for q in range(4)] + [
                e.bn_aggr(mv[0:rn, :], stats[0:rn, :, :]),
                e.tensor_scalar(mv[0:rn, 1:2], mv[0:rn, 1:2], float(LN_EPS), None, op0=ALU.add)])(rn),
                reads=['x1t', 'h2f0', 'h2f1', 'h2f2', 'h2f3', 'Eld'], writes=['x1t', 'stats'])
            P.act((lambda rn: lambda e: e.activation(mv[0:rn, 1:2], mv[0:rn, 1:2], AF.Sqrt))(rn), reads=['stats'], writes=['stats'])
            P.dve((lambda rn: lambda e: [
                e.reciprocal(mv[0:rn, 1:2], mv[0:rn, 1:2]),
                e.tensor_scalar(x1t[0:rn, :], x1t[0:rn, :], mv[0:rn, 0:1], mv[0:rn, 1:2], op0=ALU.subtract, op1=ALU.mult),
                e.tensor_tensor(x1t[0:rn, :], x1t[0:rn, :], lng[0:rn, :], op=ALU.mult),
                e.tensor_tensor(x1t[0:rn, :], x1t[0:rn, :], lnb[0:rn, :], op=ALU.add)])(rn),
                reads=['x1t', 'xt_s', 'Eld'], writes=['x1t', 'stats'])
            P.dma('sp', (lambda r0, rn: lambda e: [e.dma_start(out=x1_scr[r0:r0 + rn, :], in_=x1t[0:rn, :])])(r0, rn),
                  1, 'x1st', reads=['x1t'], writes=['x1scr%d' % ti])
            if debug:
                P.dma('sp', (lambda r0, rn: lambda e: [e.dma_start(out=dbg['x1'][r0:r0 + rn, :], in_=x1t[0:rn, :])])(r0, rn),
                      1, 'x1dbg', reads=['x1t'], writes=['dbg_x1%d' % ti])
                outs_keys.append('dbg_x1%d' % ti)
            for kq in range(4):
                bk = 4 + (kq % 2)
                P.pe((lambda kq, bk, rn: lambda e: [e.transpose(pb[bk][:, j * 128:j * 128 + rn], x1t[0:rn, (kq * 4 + j) * 128:(kq * 4 + j + 1) * 128], ident[0:rn, 0:rn])
                                                    for j in range(4)])(kq, bk, rn),
                     reads=['x1t', 'cst'], writes=['pb%d' % bk])
                P.act((lambda kq, bk, rn, w: lambda e: [e.activation(h2f[:, kq * 4 + j, 0:rn], pb[bk][:, j * 128:j * 128 + rn], AF.Identity,
                                                                      bias=modpp[:, 2, kq * 4 + j, w:w + 1], scale=modpp[:, 3, kq * 4 + j, w:w + 1])
                                                        for j in range(4)])(kq, bk, rn, w),
                      reads=['pb%d' % bk, 'modpp'], writes=['h2f%d' % kq])
            P.dma('sp', (lambda r0, rn: lambda e: [e.dma_start(out=h2T_scr[:, :, r0:r0 + rn], in_=h2f[:, :, 0:rn])])(r0, rn),
                  1, 'h2st', reads=['h2f%d' % k for k in range(4)], writes=['h2scr%d' % ti])
            P.pe((lambda rn: lambda e: [e.matmul(pb[6][0:rn, 0:NE], lhsT=h2f[:, kd, 0:rn], rhs=wr_s[:, kd, :], start=(kd == 0), stop=(kd == 15)) for kd in range(16)])(rn),
                 reads=['h2f%d' % k for k in range(4)] + ['Eld'], writes=['pb6'])
            P.dve((lambda ti, rn: lambda e: [
                e.tensor_tensor(lg[0:rn, :], pb[6][0:rn, 0:NE], brt[0:rn, :], op=ALU.add),
                e.max(top8[0:rn, :], lg[0:rn, :]),
                e.tensor_scalar(gates[0:rn, ti, :], lg[0:rn, :], top8[0:rn, 3:4], None, op0=ALU.is_ge),
                e.tensor_scalar(lg[0:rn, :], lg[0:rn, :], top8[0:rn, 0:1], None, op0=ALU.subtract)])(ti, rn),
                reads=['pb6', 'Eld'], writes=['lg', 'gates'])
            P.act((lambda rn: lambda e: e.activation(lg[0:rn, :], lg[0:rn, :], AF.Exp))(rn), reads=['lg'], writes=['lg'])
            P.dve((lambda ti, rn: lambda e: [
                e.tensor_tensor(gates[0:rn, ti, :], gates[0:rn, ti, :], lg[0:rn, :], op=ALU.mult),
                e.reduce_sum(mv[0:rn, 0:1], gates[0:rn, ti, :], axis=mybir.AxisListType.X),
                e.reciprocal(mv[0:rn, 0:1], mv[0:rn, 0:1]),
                e.tensor_scalar(gates[0:rn, ti, :], gates[0:rn, ti, :], mv[0:rn, 0:1], None, op0=ALU.mult)])(ti, rn),
                reads=['lg', 'gates', 'stats'], writes=['gates', 'stats'])
            if debug:
                P.dma('sp', (lambda ti, r0, rn: lambda e: [e.dma_start(out=dbg['gates'][r0:r0 + rn, :], in_=gates[0:rn, ti, :])])(ti, r0, rn),
                      1, 'gdbg', reads=['gates'], writes=['dbg_g%d' % ti])
                outs_keys.append('dbg_g%d' % ti)
        if with_moe:
            moe_phase(locals())
        P.finalize(final_reads=outs_keys)
    return nc


def moe_phase(L):
    P = L['P']; pb = L['pb']; pd = L['pd']; carve = L['carve']; gates = L['gates']; ident = L['ident']
    w_gu = L['w_gu']; w_dn = L['w_dn']; b_guT = L['b_guT']; b_dn = L['b_dn']
    h2T_scr = L['h2T_scr']; x1_scr = L['x1_scr']; modrow = L['modrow']; lnrows = L['lnrows']
    y_out = L['y_out']; outs_keys = L['outs_keys']; stats = L['stats']; mv = L['mv']
    NEXP = KNOB.get('nexp', NE)
    P.barrier()
    h2Th = carve(0, 8320, BF16).rearrange("p (k n) -> p k n", k=16)
    acc = carve(8320, 18432).rearrange("p (t n) -> p t n", t=9)
    actT = [carve(26752 + i * 520, 520, BF16) for i in range(4)]
    wgu = [carve(28832 + i * 2048, 2048, BF16).rearrange("p (k n) -> p k n", k=16) for i in range(3)]
    wdn = [carve(34976 + i * 1024, 1024, BF16) for i in range(4)]
    gcs = [carve(39072 + i * 512, 512) for i in range(2)]
    sgs = [carve(40096 + i * 512, 512) for i in range(2)]
    u1s = [carve(41120, 512), carve(41120, 512)]
    evts = [carve(41632, 1024), carve(41632, 1024)]
    bgu = carve(42656, 1024).rearrange("p (e j) -> p e j", e=NE)
    FB = 26752
    x1f = carve(FB, 2048)
    g2b = carve(FB + 2048, 2048)
    l2g = carve(FB + 4096, 2048)
    l2b = carve(FB + 6144, 2048)
    bdn = carve(FB + 8192, 2048)
    gT = carve(FB + 10240, 128)
    P.dma('sp', lambda e: [e.dma_start(out=bgu, in_=b_guT[:, :, :])], 1, 'bgu', writes=['bgu'])
    P.dve(lambda e: e.tensor_scalar(bgu[:, :, 16:32], bgu[:, :, 16:32], 1.0, None, op0=ALU.add), reads=['bgu'], writes=['bgu'])
    for hf in range(2):
        T0 = hf * 1024
        TH = 1024 if hf == 0 else 1040
        chunks_h = [(0, 512), (512, 512)] + ([(1024, 16)] if hf else [])
        tiles_h = [(i * 128, 128) for i in range(8)] + ([(1024, 16)] if hf else [])
        P.barrier()
        P.dma('pool', (lambda T0, TH: lambda e: [e.dma_start(out=h2Th[:, 4 * i:4 * i + 4, 0:TH], in_=h2T_scr[:, 4 * i:4 * i + 4, T0:T0 + TH]) for i in range(4)])(T0, TH),
              4, 'h2Th', writes=['h2Th'])
        P.pool(lambda e: e.memset(acc, 0.0), writes=['acc%d_%d' % (t, h) for t in range(9) for h in range(2)])
        steps = [(ex, f) for ex in range(NEXP) for f in range(16)]
        cnt = {'ev': 0, 'dv': 0}

        def prefetch_gu(si):
            ex, f = steps[si]
            sl = si % 3
            P.dma('pool', (lambda ex, f, sl: lambda e: [
                e.dma_start(out=wgu[sl][:, :, 0:128], in_=w_gu[ex][:, f * 128:(f + 1) * 128].rearrange("(k p) c -> p k c", p=128)),
                e.dma_start(out=wgu[sl][:, :, 128:256], in_=w_gu[ex][:, D + f * 128:D + (f + 1) * 128].rearrange("(k p) c -> p k c", p=128))])(ex, f, sl),
                2, 'wgu%d' % sl, writes=['wgu%d' % sl])

        def prefetch_dn(si):
            ex, f = steps[si]
            sl = si % 4
            P.dma('pool', (lambda ex, f, sl: lambda e: [e.dma_start(out=wdn[sl], in_=w_dn[ex][f * 128:(f + 1) * 128, :])])(ex, f, sl),
                  1, 'wdn%d' % sl, writes=['wdn%d' % sl])

        def gu_parts(si):
            ex, f = steps[si]
            sl = si % 3
            ab = si % 4
            parts = []
            for (c0, cn) in chunks_h:
                c2 = cnt['ev'] % 2
                pg, pu = (0, 1) if c2 == 0 else (2, 3)
                cnt['ev'] += 1
                gc, sg, u1 = gcs[c2], sgs[c2], u1s[c2]

                def mk(bank, col0, k0, k1, sl=sl, c0=c0, cn=cn):
                    def em():
                        P.pe(lambda e: [e.matmul(pb[bank][:, 0:cn], lhsT=wgu[sl][:, kd, col0:col0 + 128], rhs=h2Th[:, kd, c0:c0 + cn],
                                                 start=(kd == 0), stop=(kd == 15)) for kd in range(k0, k1)],
                             reads=['wgu%d' % sl, 'h2Th'], writes=['pb%d' % bank])
                    return em

                def mk_act(ex=ex, f=f, pg=pg, pu=pu, c0=c0, cn=cn, c2=c2, gc=gc, sg=sg, u1=u1, ab=ab):
                    def em():
                        P.dve(lambda e: e.tensor_scalar(gc[:, 0:cn], pb[pg][:, 0:cn], bgu[:, ex, f:f + 1], 7.0, op0=ALU.add, op1=ALU.min),
                              reads=['pb%d' % pg, 'bgu'], writes=['gc%d' % c2])
                        P.act(lambda e: e.activation(sg[:, 0:cn], gc[:, 0:cn], AF.Sigmoid, scale=1.702), reads=['gc%d' % c2], writes=['sg%d' % c2])
                        P.dve(lambda e: e.tensor_scalar(u1[:, 0:cn], pb[pu][:, 0:cn], bgu[:, ex, 16 + f:17 + f], 8.0, op0=ALU.add, op1=ALU.min),
                              reads=['pb%d' % pu, 'bgu'], writes=['u1'])
                        (P.pool if KNOB.get('tt_pool', True) else P.dve)(lambda e: e.tensor_tensor(sg[:, 0:cn], gc[:, 0:cn], sg[:, 0:cn], op=ALU.mult),
                              reads=['gc%d' % c2, 'sg%d' % c2], writes=['sg%d' % c2])
                        P.dve(lambda e: e.scalar_tensor_tensor(actT[ab][:, c0:c0 + cn], in0=u1[:, 0:cn], scalar=-6.0, in1=sg[:, 0:cn], op0=ALU.max, op1=ALU.mult),
                              reads=['u1', 'sg%d' % c2], writes=['actT%d' % ab])
                    return em
                if cn >= 512:
                    subs = [mk(pg, 0, 0, 8), mk(pg, 0, 8, 16), mk(pu, 128, 0, 8), mk(pu, 128, 8, 16)]
                else:
                    subs = [mk(pg, 0, 0, 16), mk(pu, 128, 0, 16)]
                act_em = mk_act()
                last = subs[-1]
                subs[-1] = (lambda last=last, act_em=act_em: (last(), act_em()))
                parts += subs
            return parts

        def down_parts(s0):
            ex, f = steps[s0]
            assert steps[s0 + 1][0] == ex
            parts = []
            order = [0, 6, 1, 2, 3, 7, 4, 5] + ([8] if len(tiles_h) > 8 else [])
            glist = []
            for tl in order:
                glist.append((tl, 0))
            for tl in order:
                glist.append((tl, 1))
            for (tl, hq) in glist:
                r0l, rn = tiles_h[tl]
                ti = hf * 8 + tl

                def em(tl=tl, r0l=r0l, rn=rn, ti=ti, hq=hq, ex=ex, s0=s0):
                    g = cnt['dv'] % 2
                    cnt['dv'] += 1
                    P.pe(lambda e: [e.matmul(pd[g][0:rn, j * 512:(j + 1) * 512], lhsT=actT[(s0 + d) % 4][:, r0l:r0l + rn],
                                             rhs=wdn[(s0 + d) % 4][:, (2 * hq + j) * 512:(2 * hq + j + 1) * 512], start=(d == 0), stop=(d == 1))
                                    for j in range(2) for d in range(2)],
                         reads=['actT%d' % (s0 % 4), 'actT%d' % ((s0 + 1) % 4), 'wdn%d' % (s0 % 4), 'wdn%d' % ((s0 + 1) % 4)], writes=['pd%d' % g])
                    asl = acc[0:rn, tl, hq * 1024:(hq + 1) * 1024]
                    if tl in (6, 7) and not KNOB.get('no_offload'):
                        P.act(lambda e: e.activation(evts[0][0:rn, :], pd[g][0:rn, :], AF.Copy, scale=gates[0:rn, ti, ex:ex + 1]),
                              reads=['pd%d' % g, 'gates'], writes=['evt'])
                        P.pool(lambda e: e.tensor_tensor(asl, asl, evts[0][0:rn, :], op=ALU.add), reads=['evt'], writes=['acc%d_%d' % (tl, hq)])
                    else:
                        P.dve(lambda e: e.scalar_tensor_tensor(asl, in0=pd[g][0:rn, :], scalar=gates[0:rn, ti, ex:ex + 1], in1=asl, op0=ALU.mult, op1=ALU.add),
                              reads=['pd%d' % g, 'gates'], writes=['acc%d_%d' % (tl, hq)])
                parts.append(em)
            return parts

        cnt['eb'] = 0
        assert len(steps) % 2 == 0
        for s0 in range(min(2, len(steps))):
            prefetch_gu(s0)
        for s0 in range(min(4, len(steps))):
            prefetch_dn(s0)
        pending = []
        quota = 0
        for si in range(len(steps)):
            if si + 2 < len(steps):
                prefetch_gu(si + 2)
            gp = gu_parts(si)
            if si % 2 == 0:
                quota = -(-len(pending) // 2)
            else:
                quota = len(pending)
            per = -(-quota // len(gp)) if quota else 0
            done = 0
            for part in gp:
                part()
                for _ in range(per):
                    if pending and done < quota:
                        pending.pop(0)()
                        done += 1
            while pending and done < quota:
                pending.pop(0)()
                done += 1
            if si % 2 == 1:
                assert not pending
                for sn in (si + 1, si + 2):
                    if 4 <= sn < len(steps):
                        prefetch_dn(sn)
                pending = down_parts(si - 1)
        while pending:
            pending.pop(0)()
        P.barrier()
        P.dma('sp', lambda e: [e.dma_start(out=g2b, in_=modrow[2:3, :].to_broadcast([128, D])),
                               e.dma_start(out=l2g, in_=lnrows[2:3, :].to_broadcast([128, D])),
                               e.dma_start(out=l2b, in_=lnrows[3:4, :].to_broadcast([128, D])),
                               e.dma_start(out=bdn[0:NE, :], in_=b_dn[:, :])], 4, 'fin', writes=['fin', 'g2b'])
        for tl, (r0l, rn) in enumerate(tiles_h):
            ti = hf * 8 + tl
            r0 = T0 + r0l
            if ti == 16:
                P.dma('sp', lambda e: [e.dma_start(out=g2b, in_=modrow[3:4, :].to_broadcast([128, D]))], 1, 'fin2', writes=['g2b'])
            P.dma('sp', (lambda r0, rn: lambda e: [e.dma_start(out=x1f[0:rn, :], in_=x1_scr[r0:r0 + rn, :])])(r0, rn), 1, 'x1f', writes=['x1f'])
            P.pe((lambda ti, rn: lambda e: e.transpose(pb[0][0:NE, 0:rn], gates[0:rn, ti, :], ident[0:rn, 0:rn]))(ti, rn), reads=['gates', 'cst'], writes=['pb0'])
            P.act((lambda rn: lambda e: e.copy(gT[0:NE, 0:rn], pb[0][0:NE, 0:rn]))(rn), reads=['pb0'], writes=['gT'])
            for q in range(4):
                P.pe((lambda rn, q: lambda e: e.matmul(pb[4 + q][0:rn, :], lhsT=gT[0:NE, 0:rn], rhs=bdn[0:NE, q * 512:(q + 1) * 512], start=True, stop=True))(rn, q),
                     reads=['gT', 'fin'], writes=['pb%d' % (4 + q)])
                P.dve((lambda tl, rn, q: lambda e: [
                    e.tensor_tensor(acc[0:rn, tl, q * 512:(q + 1) * 512], acc[0:rn, tl, q * 512:(q + 1) * 512], pb[4 + q][0:rn, :], op=ALU.add)])(tl, rn, q),
                    reads=['pb%d' % (4 + q)], writes=['acc%d_%d' % (tl, q // 2)])
            P.dve((lambda tl, rn: lambda e: [
                e.tensor_tensor(acc[0:rn, tl, :], acc[0:rn, tl, :], g2b[0:rn, :], op=ALU.mult),
                e.scalar_tensor_tensor(x1f[0:rn, :], in0=x1f[0:rn, :], scalar=float(DN_ALPHA), in1=acc[0:rn, tl, :], op0=ALU.mult, op1=ALU.add),
            ] + [e.bn_stats(stats[0:rn, q, :], x1f[0:rn, q * 512:(q + 1) * 512]) for q in range(4)] + [
                e.bn_aggr(mv[0:rn, :], stats[0:rn, :, :]),
                e.tensor_scalar(mv[0:rn, 1:2], mv[0:rn, 1:2], float(LN_EPS), None, op0=ALU.add)])(tl, rn),
                reads=['x1f', 'g2b', 'fin'], writes=['x1f', 'acc%d_0' % tl, 'acc%d_1' % tl, 'stats'])
            P.act((lambda rn: lambda e: e.activation(mv[0:rn, 1:2], mv[0:rn, 1:2], AF.Sqrt))(rn), reads=['stats'], writes=['stats'])
            P.dve((lambda rn: lambda e: [
                e.reciprocal(mv[0:rn, 1:2], mv[0:rn, 1:2]),
                e.tensor_scalar(x1f[0:rn, :], x1f[0:rn, :], mv[0:rn, 0:1], mv[0:rn, 1:2], op0=ALU.subtract, op1=ALU.mult),
                e.tensor_tensor(x1f[0:rn, :], x1f[0:rn, :], l2g[0:rn, :], op=ALU.mult),
                e.tensor_tensor(x1f[0:rn, :], x1f[0:rn, :], l2b[0:rn, :], op=ALU.add)])(rn),
                reads=['stats', 'x1f', 'fin'], writes=['x1f', 'stats'])
            P.dma('sp', (lambda r0, rn: lambda e: [e.dma_start(out=y_out[r0:r0 + rn, :], in_=x1f[0:rn, :])])(r0, rn), 1, 'yst', reads=['x1f'], writes=['y%d' % ti])
            outs_keys.append('y%d' % ti)


def prep_shared(inp):
    f = np.float32
    d = {}
    d['w_ada'] = np.ascontiguousarray(inp['w_ada'][0], dtype=f)
    ba = np.asarray(inp['b_ada'][0], dtype=f)
    d['b_adaT'] = np.ascontiguousarray(ba.reshape(96, 128).T)
    d['b_ada_row'] = np.ascontiguousarray(ba.reshape(1, -1))
    d['w_in'] = np.ascontiguousarray(inp['w_in'][0], dtype=f)
    d['w_out'] = np.ascontiguousarray(inp['w_out'][0], dtype=f)
    d['w_pool'] = np.ascontiguousarray(inp['w_pool'][0], dtype=f)
    d['pool_scaleT'] = np.ascontiguousarray(np.asarray(inp['pool_scale'][0], dtype=f).reshape(8, 128).T)
    lr = np.asarray(inp['lambda_re'][0], dtype=f).T
    li = np.asarray(inp['lambda_im'][0], dtype=f).T
    lam = np.stack([np.concatenate([lr, lr], 0), np.concatenate([li, li], 0)], 1)
    d['lamst'] = np.ascontiguousarray(lam)
    d['logdt_row'] = np.ascontiguousarray(np.asarray(inp['log_dt'][0], dtype=f).reshape(1, 64))
    br = np.asarray(inp['ssm_b_re'][0], dtype=f).transpose(1, 0, 2)
    bi = np.asarray(inp['ssm_b_im'][0], dtype=f).transpose(1, 0, 2)
    d['Bst'] = np.ascontiguousarray(np.stack([np.concatenate([br, bi], 0), np.concatenate([bi, br], 0)], 0).reshape(2, 128, 1024))
    cr = np.asarray(inp['ssm_c_re'][0], dtype=f).transpose(2, 0, 1)
    ci = np.asarray(inp['ssm_c_im'][0], dtype=f).transpose(2, 0, 1)
    d['Cst'] = np.ascontiguousarray(np.stack([np.concatenate([cr, ci], 0), np.concatenate([ci, cr], 0)], 0).reshape(2, 128, 1024))
    d['dskipT'] = np.ascontiguousarray(np.asarray(inp['d_skip'][0], dtype=f).reshape(8, 128).T)
    d['bgluT'] = np.ascontiguousarray(np.asarray(inp['b_glu'][0], dtype=f).reshape(8, 128).T)
    wg = np.asarray(inp['w_glu'][0], dtype=f)
    bd = np.zeros((8, 128, 128), f)
    for g in range(64):
        q = g % 8
        bd[g // 8, q * 16:(q + 1) * 16, q * 16:(q + 1) * 16] = wg[g]
    d['wglu_bd'] = bd
    d['lnrows'] = np.ascontiguousarray(np.stack([inp['ln1_g'][0], inp['ln1_b'][0], inp['ln2_g'][0], inp['ln2_b'][0]], 0), dtype=f)
    d['w_router'] = np.ascontiguousarray(inp['w_router'][0], dtype=f)
    d['b_router'] = np.ascontiguousarray(np.asarray(inp['b_router'][0], dtype=f).reshape(1, NE))
    c = np.zeros((128, 153), f)
    c[:, 0:128] = np.eye(128, dtype=f)
    c[:, 128:144] = 1.0 / (np.arange(16, dtype=f) + 1.0)
    for p in range(128):
        c[p, 144 + p // 16] = 1.0
    c[:64, 152] = 1.0
    c[64:, 152] = -1.0
    d['consts'] = c
    return d


def prep_moe_shared(inp):
    f = np.float32
    d = {}
    d['w_gu'] = np.ascontiguousarray(inp['w_gate_up'][0], dtype=f)
    d['w_dn'] = np.ascontiguousarray(inp['w_down'][0], dtype=f)
    bgu = np.asarray(inp['b_gate_up'][0], dtype=f)
    d['b_guT'] = np.ascontiguousarray(bgu.reshape(NE, 32, 128).transpose(2, 0, 1))
    d['b_dn'] = np.ascontiguousarray(inp['b_down'][0], dtype=f)
    return d


def prep_core(inp, b):
    f = np.float32
    d = {}
    xtok = np.concatenate([np.asarray(inp['x_prompt'][b], dtype=f), np.asarray(inp['x_sample'][b], dtype=f)], 0)
    d['xtok'] = np.ascontiguousarray(xtok)
    d['xT'] = np.ascontiguousarray(xtok.T)
    c2 = np.stack([np.asarray(inp['c_prompt'][b], dtype=f), np.asarray(inp['c_sample'][b], dtype=f)], -1)
    d['cT'] = np.ascontiguousarray(c2.reshape(16, 128, 2).transpose(1, 0, 2))
    d['cache_poolT'] = np.ascontiguousarray(np.asarray(inp['cache_pool'][0, b], dtype=f).T)
    sr = np.asarray(inp['state_ssm_re'][0, b], dtype=f).T
    si = np.asarray(inp['state_ssm_im'][0, b], dtype=f).T
    d['h0st'] = np.ascontiguousarray(np.concatenate([sr, si], 0))
    return d


_NC_CACHE = {}


def kernel(**inputs):
    n = 8
    if 'nc' not in _NC_CACHE:
        _NC_CACHE['nc'] = build_nc(debug=False, with_moe=True)
    nc = _NC_CACHE['nc']
    shared = prep_shared(inputs)
    shared.update(prep_moe_shared(inputs))
    in_maps = []
    for b in range(n):
        m = dict(shared)
        m.update(prep_core(inputs, b))
        in_maps.append(m)
    res = run_bass_kernel_spmd(nc, in_maps, core_ids=list(range(n)))
    R = res.results
    y = np.stack([r['y'] for r in R], 0)
    y_prompt = np.ascontiguousarray(y[:, :NP_, :])
    y_sample = np.ascontiguousarray(y[:, NP_:, :])
    npool = np.stack([r['npool'] for r in R], 0)
    nssm = np.stack([r['nssm'] for r in R], 0)
    new_pool_p = np.ascontiguousarray(npool[None, :, 0])
    new_pool_s = np.ascontiguousarray(npool[None, :, 1])
    re_p = np.ascontiguousarray(nssm[None, :, 0, :, 0:64])
    im_p = np.ascontiguousarray(nssm[None, :, 0, :, 64:128])
    re_s = np.ascontiguousarray(nssm[None, :, 1, :, 0:64])
    im_s = np.ascontiguousarray(nssm[None, :, 1, :, 64:128])
    return (y_prompt, y_sample, new_pool_p, re_p, im_p, new_pool_s, re_s, im_s)
```
